# Optimizing a Trainium2 kernel written in Bass

```python
import math
import jax, jax.numpy as jnp
from jax import lax
import numpy as np

D_MODEL = 1024
BATCH = 8
SEQ = 4096
DEPTH = 2

HEAD_DIM = 64
D_MIX = D_MODEL
DN_HEADS = 6
DN_WIDTH = DN_HEADS * HEAD_DIM
FX_HEADS = 6
FX_WIDTH = FX_HEADS * HEAD_DIM
POOL_GROUPS = 4
POOL_WIDTH = D_MIX - DN_WIDTH - FX_WIDTH
POOL_GROUP_DIM = POOL_WIDTH // POOL_GROUPS
POOL_WINDOWS = (2, 4, 8, 16)
CONV_WIDTH = 4
DN_CHUNK = 64
FX_BLOCK = 128
D_FF = 2816
N_EXPERTS = 8
TOP_K = 2
D_FF_EXPERT = 1536
N_DENSE = (DEPTH + 1) // 2
N_MOE = DEPTH // 2
EPS = 1e-6
N_IN = 4 * DN_WIDTH + 2 * DN_HEADS + 3 * FX_WIDTH + FX_HEADS + POOL_WIDTH

kernel_name = 'hybrid_deltanet_fox_pool_moe'

F32 = jnp.float32


def rmsnorm(x, g):
    xf = x.astype(F32)
    y = xf * lax.rsqrt(jnp.mean(xf * xf, axis=-1, keepdims=True) + EPS)
    return (y * g.astype(F32)).astype(x.dtype)


def l2norm(x):
    return x * lax.rsqrt(jnp.sum(x * x, axis=-1, keepdims=True) + EPS)


def causal_conv(x, w):
    c = x.shape[-1]
    return lax.conv_general_dilated(x, w.astype(x.dtype)[:, None, :], window_strides=(1,),
                                    padding=[(CONV_WIDTH - 1, 0)],
                                    dimension_numbers=('NWC', 'WIO', 'NWC'),
                                    feature_group_count=c)


def chunk_gated_delta_rule(q, k, v, g, beta):
    B, T, H, Dk = q.shape
    Dv = v.shape[-1]
    C = DN_CHUNK
    N = T // C

    def to_chunks(a):
        a = a.reshape((B, N, C, H) + a.shape[3:])
        return jnp.moveaxis(a, 3, 1)

    q, k, v, g, beta = (to_chunks(a) for a in (q, k, v, g, beta))
    G = jnp.cumsum(g, axis=-1)
    causal = jnp.tril(jnp.ones((C, C), bool))
    strict = jnp.tril(jnp.ones((C, C), bool), -1)
    diff = G[..., :, None] - G[..., None, :]
    decay = jnp.where(causal, jnp.exp(jnp.where(causal, diff, 0.0)), 0.0)
    kb = k * beta[..., None]
    a_mat = jnp.where(strict, jnp.einsum('bhnid,bhnjd->bhnij', kb, k) * decay, 0.0)
    eye = jnp.eye(C, dtype=q.dtype)
    rhs = jnp.concatenate([v * beta[..., None], kb * jnp.exp(G)[..., None]], axis=-1)
    uw = lax.linalg.triangular_solve(a_mat + eye, rhs, left_side=True, lower=True,
                                     transpose_a=False, conjugate_a=False, unit_diagonal=True)
    u, w = uw[..., :Dv], uw[..., Dv:]
    qk = jnp.where(causal, jnp.einsum('bhnid,bhnjd->bhnij', q, k) * decay, 0.0)
    q_dec = q * jnp.exp(G)[..., None]
    k_dec = k * jnp.exp(G[..., -1:] - G)[..., None]
    last = jnp.exp(G[..., -1])

    def step(S, xs):
        u_n, w_n, qk_n, q_n, k_n, l_n = xs
        v_new = u_n - jnp.einsum('bhck,bhkv->bhcv', w_n, S)
        o_n = jnp.einsum('bhck,bhkv->bhcv', q_n, S) + jnp.einsum('bhij,bhjv->bhiv', qk_n, v_new)
        S = S * l_n[..., None, None] + jnp.einsum('bhck,bhcv->bhkv', k_n, v_new)
        return S, o_n

    xs = tuple(jnp.moveaxis(a, 2, 0) for a in (u, w, qk, q_dec, k_dec, last))
    S0 = jnp.zeros((B, H, Dk, Dv), q.dtype)
    _, o = lax.scan(step, S0, xs)
    return jnp.transpose(o, (1, 0, 3, 2, 4)).reshape(B, T, H, Dv)


def gated_deltanet(q, k, v, z, a, b, conv_w, a_log, dt_bias, onorm_g):
    B, T, _ = q.shape
    dt = q.dtype
    qkv = jax.nn.silu(causal_conv(jnp.concatenate([q, k, v], axis=-1), conv_w)).astype(F32)
    q, k, v = jnp.split(qkv, 3, axis=-1)
    heads = lambda t: t.reshape(B, T, DN_HEADS, HEAD_DIM)
    q = l2norm(heads(q)) * (HEAD_DIM ** -0.5)
    k = l2norm(heads(k))
    v = heads(v)
    beta = jax.nn.sigmoid(b.astype(F32))
    g = -jnp.exp(a_log.astype(F32)) * jax.nn.softplus(a.astype(F32) + dt_bias.astype(F32))
    o = chunk_gated_delta_rule(q, k, v, g, beta)
    o = rmsnorm(o, onorm_g) * jax.nn.silu(heads(z).astype(F32))
    return o.reshape(B, T, DN_WIDTH).astype(dt)


def forgetting_attention(q, k, v, f, qnorm_g, knorm_g, f_bias):
    B, T, _ = q.shape
    dt = q.dtype
    heads = lambda t: t.reshape(B, T, FX_HEADS, HEAD_DIM).transpose(0, 2, 1, 3)
    q = rmsnorm(heads(q), qnorm_g)
    k = rmsnorm(heads(k), knorm_g)
    v = heads(v)
    logf = jax.nn.log_sigmoid(f.astype(F32) + f_bias.astype(F32))
    c = jnp.cumsum(logf, axis=1).transpose(0, 2, 1)
    scale = HEAD_DIM ** -0.5
    outs = []
    for i in range(T // FX_BLOCK):
        lo, hi = i * FX_BLOCK, (i + 1) * FX_BLOCK
        s = jnp.einsum('bhqd,bhkd->bhqk', q[:, :, lo:hi], k[:, :, :hi]).astype(F32) * scale
        s = s + c[:, :, lo:hi, None] - c[:, :, None, :hi]
        q_pos = jnp.arange(lo, hi)[:, None]
        k_pos = jnp.arange(hi)[None, :]
        s = jnp.where(k_pos <= q_pos, s, -jnp.inf)
        p = jax.nn.softmax(s, axis=-1).astype(dt)
        outs.append(jnp.einsum('bhqk,bhkd->bhqd', p, v[:, :, :hi]))
    o = jnp.concatenate(outs, axis=2)
    return o.transpose(0, 2, 1, 3).reshape(B, T, FX_WIDTH)


def pool_mixer(xp, pool_w, pool_scale):
    B, T, _ = xp.shape
    xg = xp.reshape(B, T, POOL_GROUPS, POOL_GROUP_DIM).astype(F32)
    cs = jnp.concatenate([jnp.zeros((B, 1, POOL_GROUPS, POOL_GROUP_DIM), F32),
                          jnp.cumsum(xg, axis=1)], axis=1)
    win = jnp.array(POOL_WINDOWS, jnp.int32)
    t1 = jnp.arange(1, T + 1, dtype=jnp.int32)[:, None]
    start = jnp.maximum(t1 - win[None, :], 0)
    count = (t1 - start).astype(F32)
    gidx = jnp.arange(POOL_GROUPS)[None, :]
    window_sum = cs[:, 1:] - cs[:, start, gidx]
    y = (window_sum / count[..., None] - xg).astype(xp.dtype)
    y = jnp.einsum('btgc,gcd->btgd', y, pool_w)
    return y.reshape(B, T, POOL_WIDTH) * pool_scale


def hybrid_mixer(h, w_in, dn_conv, dn_a_log, dn_dt_bias, dn_onorm, fx_qnorm, fx_knorm, fx_f_bias,
                 pool_w, pool_scale, w_out):
    proj = h @ w_in
    sizes = (DN_WIDTH, DN_WIDTH, DN_WIDTH, DN_WIDTH, DN_HEADS, DN_HEADS,
             FX_WIDTH, FX_WIDTH, FX_WIDTH, FX_HEADS, POOL_WIDTH)
    (dn_q, dn_k, dn_v, dn_z, dn_a, dn_b, fx_q, fx_k, fx_v, fx_f, pl_x) = jnp.split(
        proj, np.cumsum(sizes)[:-1].tolist(), axis=-1)
    o_dn = gated_deltanet(dn_q, dn_k, dn_v, dn_z, dn_a, dn_b, dn_conv, dn_a_log, dn_dt_bias, dn_onorm)
    o_fx = forgetting_attention(fx_q, fx_k, fx_v, fx_f, fx_qnorm, fx_knorm, fx_f_bias)
    o_pl = pool_mixer(pl_x, pool_w, pool_scale)
    return jnp.concatenate([o_dn, o_fx, o_pl], axis=-1) @ w_out


def swiglu(h, w_gate, w_up, w_down):
    return (jax.nn.silu(h @ w_gate) * (h @ w_up)) @ w_down


def moe_swiglu(h, router, w_gate, w_up, w_down):
    B, T, D = h.shape
    ht = h.reshape(B * T, D)
    probs = jax.nn.softmax((ht @ router).astype(F32), axis=-1)
    top_p, top_i = lax.top_k(probs, TOP_K)
    top_p = top_p / jnp.sum(top_p, axis=-1, keepdims=True)
    gates = jnp.sum(jax.nn.one_hot(top_i, N_EXPERTS, dtype=F32) * top_p[..., None], axis=1).astype(h.dtype)
    out = jnp.zeros_like(ht)
    for e in range(N_EXPERTS):
        out = out + gates[:, e:e + 1] * swiglu(ht, w_gate[e], w_up[e], w_down[e])
    return out.reshape(B, T, D)


def setup_inputs(seed: int = 0) -> dict:
    key = jax.random.key(seed)
    ks = list(jax.random.split(key, 32))
    counter = [0]

    def nk():
        counter[0] += 1
        return ks[counter[0] - 1]

    def nrm(shape, scale):
        return scale * jax.random.normal(nk(), shape, F32)

    def gain(shape):
        return 1.0 + nrm(shape, 0.05)

    L = DEPTH
    x = nrm((BATCH, SEQ, D_MODEL), 1.0)
    norm1 = gain((L, D_MODEL))
    w_in = nrm((L, D_MODEL, N_IN), D_MODEL ** -0.5)
    dn_conv = nrm((L, CONV_WIDTH, 3 * DN_WIDTH), CONV_WIDTH ** -0.5)
    dn_a_log = jnp.log(jax.random.uniform(nk(), (L, DN_HEADS), F32, 1.0, 16.0))
    dt0 = jnp.exp(jax.random.uniform(nk(), (L, DN_HEADS), F32, math.log(1e-3), math.log(1e-1)))
    dn_dt_bias = dt0 + jnp.log(-jnp.expm1(-dt0))
    dn_onorm = gain((L, HEAD_DIM))
    fx_qnorm = gain((L, HEAD_DIM))
    fx_knorm = gain((L, HEAD_DIM))
    fx_f_bias = 2.0 + nrm((L, FX_HEADS), 0.5)
    pool_w = nrm((L, POOL_GROUPS, POOL_GROUP_DIM, POOL_GROUP_DIM), POOL_GROUP_DIM ** -0.5)
    pool_scale = 1.0 + nrm((L, POOL_WIDTH), 0.1)
    w_out = nrm((L, D_MIX, D_MODEL), 0.5 * D_MIX ** -0.5)
    norm2 = gain((L, D_MODEL))
    ffn_gate = nrm((N_DENSE, D_MODEL, D_FF), D_MODEL ** -0.5)
    ffn_up = nrm((N_DENSE, D_MODEL, D_FF), D_MODEL ** -0.5)
    ffn_down = nrm((N_DENSE, D_FF, D_MODEL), 0.5 * D_FF ** -0.5)
    router = nrm((N_MOE, D_MODEL, N_EXPERTS), D_MODEL ** -0.5)
    moe_gate = nrm((N_MOE, N_EXPERTS, D_MODEL, D_FF_EXPERT), D_MODEL ** -0.5)
    moe_up = nrm((N_MOE, N_EXPERTS, D_MODEL, D_FF_EXPERT), D_MODEL ** -0.5)
    moe_down = nrm((N_MOE, N_EXPERTS, D_FF_EXPERT, D_MODEL), 0.5 * D_FF_EXPERT ** -0.5)
    return {'x': x, 'norm1': norm1, 'w_in': w_in, 'dn_conv': dn_conv, 'dn_a_log': dn_a_log,
            'dn_dt_bias': dn_dt_bias, 'dn_onorm': dn_onorm, 'fx_qnorm': fx_qnorm, 'fx_knorm': fx_knorm,
            'fx_f_bias': fx_f_bias, 'pool_w': pool_w, 'pool_scale': pool_scale, 'w_out': w_out,
            'norm2': norm2, 'ffn_gate': ffn_gate, 'ffn_up': ffn_up, 'ffn_down': ffn_down,
            'router': router, 'moe_gate': moe_gate, 'moe_up': moe_up, 'moe_down': moe_down}


def reference(x, norm1, w_in, dn_conv, dn_a_log, dn_dt_bias, dn_onorm, fx_qnorm, fx_knorm, fx_f_bias,
              pool_w, pool_scale, w_out, norm2, ffn_gate, ffn_up, ffn_down, router, moe_gate, moe_up,
              moe_down):
    for layer in range(DEPTH):
        h = rmsnorm(x, norm1[layer])
        x = x + hybrid_mixer(h, w_in[layer], dn_conv[layer], dn_a_log[layer], dn_dt_bias[layer],
                             dn_onorm[layer], fx_qnorm[layer], fx_knorm[layer], fx_f_bias[layer],
                             pool_w[layer], pool_scale[layer], w_out[layer])
        h = rmsnorm(x, norm2[layer])
        j = layer // 2
        if layer % 2 == 0:
            x = x + swiglu(h, ffn_gate[j], ffn_up[j], ffn_down[j])
        else:
            x = x + moe_swiglu(h, router[j], moe_gate[j], moe_up[j], moe_down[j])
    return x
```

```python
import contextlib
import os
import numpy as np
import concourse.bass as bass
import concourse.mybir as mybir
from concourse.bass_utils import run_bass_kernel_spmd

F32 = mybir.dt.float32
BF16 = mybir.dt.bfloat16
AF = mybir.ActivationFunctionType
ALU = mybir.AluOpType
AX = mybir.AxisListType

T = 4096
D = 1024
NSB = 8
SB = 512
SBM = 256
NSBM = T // SBM
NTB = SBM // 128
NEG = -30000.0
EPS = 1e-6
EPOCH = 30000
SAME_ENGINE_SYNC = True


class Buf:
    __slots__ = ("w", "r")

    def __init__(self):
        self.w = []
        self.r = []


class Sched:
    ENGS = ("pe", "dve", "act", "pool", "sp")

    def __init__(self, nc, stack, n_dma_sems=12):
        self.nc = nc
        self.stack = stack
        self.streams = {e: [] for e in self.ENGS}
        self.sem = {}
        self.cnt = {}
        self.waited = {e: {} for e in self.ENGS}
        self.nsem = 0
        for e in self.ENGS:
            self._new_epoch(e)
        self.dma_pool = {}
        self.n_dma_sems = n_dma_sems
        self.last = {e: None for e in self.ENGS}
        self.nops = 0

    def _alloc_sem(self, name):
        self.nsem += 1
        return self.stack.enter_context(self.nc.semaphore(name))

    def _new_epoch(self, e):
        self.sem[e] = self._alloc_sem(f"tl_{e}_{self.nsem}")
        self.cnt[e] = 0

    def _emit_waits(self, eng, deps):
        need = {}
        for tok in deps:
            if tok is None:
                continue
            sem, val, src = tok
            if src == eng and (eng == "pe" or not SAME_ENGINE_SYNC):
                continue
            k = id(sem)
            if k not in need or need[k][1] < val:
                need[k] = (sem, val)
        w = self.waited[eng]
        for k, (sem, val) in need.items():
            if w.get(k, 0) >= val:
                continue
            w[k] = val
            self.streams[eng].append(("wait", sem, val))

    @staticmethod
    def _deps(reads, writes):
        deps = []
        for b in reads:
            deps.extend(b.w)
        for b in writes:
            deps.extend(b.w)
            deps.extend(b.r)
        return deps

    @staticmethod
    def _mark(tok, reads, writes, add=False):
        for b in reads:
            b.r.append(tok)
            if len(b.r) > 24:
                b.r = b.r[-24:]
        for b in writes:
            if add:
                b.w.append(tok)
            else:
                b.w = [tok]
                b.r = []

    def op(self, eng, fn, reads=(), writes=()):
        self._emit_waits(eng, self._deps(reads, writes))
        if self.cnt[eng] >= EPOCH:
            self._new_epoch(eng)
        self.cnt[eng] += 1
        tok = (self.sem[eng], self.cnt[eng], eng)
        self.streams[eng].append(("op", fn, self.sem[eng], 1))
        self.last[eng] = tok
        self._mark(tok, reads, writes)
        self.nops += 1
        return tok

    def dma(self, q, out, in_, reads=(), writes=(), add=False):
        deps = self._deps(reads, () if add else writes)
        if add:
            for b in writes:
                deps.extend(b.r)
        if q not in self.dma_pool:
            self.dma_pool[q] = {"sems": [self._alloc_sem(f"dma_{q}_{i}") for i in range(self.n_dma_sems)],
                                "vals": [0] * self.n_dma_sems, "i": 0}
        p = self.dma_pool[q]
        i = p["i"]
        p["i"] = (i + 1) % self.n_dma_sems
        if p["vals"][i] + 16 > EPOCH:
            p["sems"][i] = self._alloc_sem(f"dma_{q}_{i}_{self.nsem}")
            p["vals"][i] = 0
        sem = p["sems"][i]
        if p["vals"][i] > 0:
            deps.append((sem, p["vals"][i], "dma"))
        self._emit_waits(q, deps)
        p["vals"][i] += 16
        tok = (sem, p["vals"][i], "dma")
        self.streams[q].append(("op", lambda e, out=out, in_=in_: e.dma_start(out=out, in_=in_), sem, 16))
        self._mark(tok, reads, writes, add=add)
        return tok

    def barrier(self):
        toks = [t for t in self.last.values() if t is not None]
        for q, p in self.dma_pool.items():
            for s, v in zip(p["sems"], p["vals"]):
                if v > 0:
                    toks.append((s, v, "dma"))
        for e in self.ENGS:
            self._emit_waits(e, toks)

    def emit(self):
        nc = self.nc
        streams = self.streams
        with nc.Block() as block:
            def run(e, name):
                for it in streams[name]:
                    if it[0] == "wait":
                        e.wait_ge(it[1], it[2])
                    else:
                        it[1](e).then_inc(it[2], it[3])

            @block.tensor
            def _(e):
                run(e, "pe")

            @block.vector
            def _(e):
                run(e, "dve")

            @block.scalar
            def _(e):
                run(e, "act")

            @block.gpsimd
            def _(e):
                run(e, "pool")

            @block.sync
            def _(e):
                run(e, "sp")
        self.streams = {e: [] for e in self.ENGS}


N_IN = 2962
SP_COLS = 256


def _layout_inputs(inp):
    out = {}
    f = lambda a: np.ascontiguousarray(a, dtype=np.float32)
    o_q, o_k, o_v, o_z, o_a, o_b = 0, 384, 768, 1152, 1536, 1542
    o_fq, o_fk, o_fv, o_ff, o_pl = 1548, 1932, 2316, 2700, 2706
    cols = np.concatenate([
        np.arange(o_q, o_q + 384), np.arange(o_k, o_k + 384), np.arange(o_v, o_v + 384),
        np.arange(o_fq, o_fq + 384), np.arange(o_fk, o_fk + 384), np.arange(o_pl, o_pl + 256),
        np.arange(o_fv, o_fv + 384), np.arange(o_z, o_z + 384),
        np.arange(o_a, o_a + 6), np.arange(o_b, o_b + 6), np.arange(o_ff, o_ff + 6)])
    wi = np.zeros((2, 1024, 3072), np.float32)
    wi[:, :, :cols.size] = inp["w_in"][:, :, cols]
    wi = wi.reshape(2, 8, 128, 6, 512).transpose(0, 3, 2, 1, 4)
    out["wi"] = f(wi.reshape(2 * 6 * 128, 8 * 512))
    wo = inp["w_out"].reshape(2, 8, 128, 1024).transpose(0, 2, 1, 3)
    out["wo"] = f(wo.reshape(2 * 128, 8 * 1024))
    g = inp["ffn_gate"][0].reshape(8, 128, 11, 256)
    u = inp["ffn_up"][0].reshape(8, 128, 11, 256)
    gu = np.stack([g, u], 0).transpose(3, 2, 0, 1, 4)
    out["wgu"] = f(gu.reshape(11 * 128, 2 * 8 * 256))
    dn = inp["ffn_down"][0].reshape(2, 11, 128, 1024).transpose(0, 2, 1, 3)
    out["wd"] = f(dn.reshape(2 * 128, 11 * 1024))
    g = inp["moe_gate"][0].reshape(8, 8, 128, 6, 256)
    u = inp["moe_up"][0].reshape(8, 8, 128, 6, 256)
    gu = np.stack([g, u], 0).transpose(1, 4, 3, 0, 2, 5)
    out["mgu"] = f(gu.reshape(8 * 6 * 128, 2 * 8 * 256))
    dn = inp["moe_down"][0].reshape(8, 12, 128, 1024).transpose(0, 2, 1, 3)
    out["md"] = f(dn.reshape(8 * 128, 12 * 1024))
    out["router"] = f(inp["router"][0].reshape(8, 128, 8).transpose(1, 0, 2).reshape(128, 64))
    sp = np.zeros((2, 128, SP_COLS), np.float32)
    bc = lambda v: np.broadcast_to(np.asarray(v)[None, :], (128, len(v)))
    for l in range(2):
        c = 0
        sp[l, :, c:c + 8] = inp["norm1"][l].reshape(8, 128).T; c += 8
        sp[l, :, c:c + 8] = inp["norm2"][l].reshape(8, 128).T; c += 8
        sp[l, :, c:c + 36] = inp["dn_conv"][l].reshape(4, 9, 128).transpose(2, 1, 0).reshape(128, 36); c += 36
        sp[l, :, c:c + 6] = bc(inp["dn_a_log"][l]); c += 6
        sp[l, :, c:c + 6] = bc(inp["dn_dt_bias"][l]); c += 6
        sp[l, :, c:c + 64] = bc(inp["dn_onorm"][l]); c += 64
        sp[l, :, c] = np.tile(inp["fx_qnorm"][l], 2); c += 1
        sp[l, :, c] = np.tile(inp["fx_knorm"][l], 2); c += 1
        sp[l, :, c:c + 6] = bc(inp["fx_f_bias"][l]); c += 6
        sp[l, :, c:c + 2] = inp["pool_scale"][l].reshape(2, 128).T; c += 2
    out["sp"] = f(sp.reshape(2 * 128, SP_COLS))
    pw = np.zeros((2, 128, 2, 128), np.float32)
    for l in range(2):
        for c in range(2):
            pw[l, 0:64, c, 0:64] = inp["pool_w"][l, 2 * c]
            pw[l, 64:128, c, 64:128] = inp["pool_w"][l, 2 * c + 1]
    out["pw"] = f(pw.reshape(2 * 128, 256))
    return out


CSTF_COLS = 128 * 4 + 4 + 32
CSTB_COLS = 128 * 9 + 2 * SBM + 128


def _constants():
    cf = np.zeros((128, CSTF_COLS), np.float32)
    cb = np.zeros((128, CSTB_COLS), np.float32)
    j = np.arange(128)[:, None]
    i = np.arange(128)[None, :]
    cf[:, 0:128] = np.eye(128)
    cf[:, 128:256] = (j <= i)
    cf[:, 256:384] = 1.0
    cf[:, 384:512] = np.where(i > j, 0.0, NEG)
    cf[:, 512] = np.where(np.arange(128) < 64, 1 / 2, 1 / 4)
    cf[:, 513] = np.where(np.arange(128) < 64, 1 / 8, 1 / 16)
    wins = np.array([[2, 4], [8, 16]])
    for ch in range(2):
        w = np.where(np.arange(128) < 64, wins[ch, 0], wins[ch, 1])[:, None]
        cf[:, 516 + 16 * ch:516 + 16 * (ch + 1)] = 1.0 / np.minimum(np.arange(16)[None, :] + 1, w)
    cb[:, 0:128] = np.eye(128)
    cb[:, 128:256] = (j // 64 == i // 64)
    o = 256
    for l in range(7):
        b = 1 << l
        m = (j // (2 * b) == i // (2 * b)) & ((j % (2 * b)) < b) & ((i % (2 * b)) >= b)
        cb[:, o:o + 128] = -1.0 * m; o += 128
    t = np.arange(SBM)[None, :]
    for m in range(NTB):
        cb[:, o:o + SBM] = (j + 128 * m <= t); o += SBM
    m0 = (j // 2 == i // 2) & ((j % 2) < 1) & ((i % 2) >= 1)
    cb[:, o:o + 128] = -1.0 * m0.T
    return cf, cb


class Prog:
    def __init__(self, debug=False, nlayers=2, stop_after=None, layers=None):
        self.debug = debug
        self.nlayers = nlayers
        self.stop_after = stop_after
        self.layers = tuple(layers) if layers is not None else tuple(range(nlayers))

    def build(self):
        nc = bass.Bass("TRN2", target_bir_lowering=False)
        self.nc = nc
        dt_in = lambda name, shape: nc.dram_tensor(name, list(shape), F32, kind="ExternalInput").ap()
        self.x_in = dt_in("x", (T, D))
        self.d_wi = dt_in("wi", (2 * 6 * 128, 4096))
        self.d_wo = dt_in("wo", (256, 8192))
        self.d_wgu = dt_in("wgu", (11 * 128, 4096))
        self.d_wd = dt_in("wd", (256, 11264))
        self.d_mgu = dt_in("mgu", (48 * 128, 4096))
        self.d_md = dt_in("md", (8 * 128, 12288))
        self.d_router = dt_in("router", (128, 64))
        self.d_sp = dt_in("sp", (256, SP_COLS))
        self.d_pw = dt_in("pw", (256, 256))
        self.d_cstf = dt_in("cstf", (128, CSTF_COLS))
        self.d_cstb = dt_in("cstb", (128, CSTB_COLS))
        self.out = nc.dram_tensor("out", [T, D], F32, kind="ExternalOutput").ap()
        kind_dbg = "ExternalOutput" if self.debug else "Internal"
        self.x1 = [nc.dram_tensor(f"x1_{l}", [T, D], F32, kind=kind_dbg).ap() for l in range(2)]
        self.x2 = nc.dram_tensor("x2_0", [T, D], F32, kind=kind_dbg).ap()
        self.dbg_g = nc.dram_tensor("dbg_g", [T, 16], F32, kind=kind_dbg).ap()
        scr = lambda name, ap: nc.dram_tensor(name, list(ap.shape), BF16, kind="Internal").ap()
        self.b_wi = scr("b_wi", self.d_wi)
        self.b_wo = scr("b_wo", self.d_wo)
        self.b_wgu = scr("b_wgu", self.d_wgu)
        self.b_wd = scr("b_wd", self.d_wd)
        self.b_mgu = scr("b_mgu", self.d_mgu)
        self.b_md = scr("b_md", self.d_md)
        self.wbuf = {}

        with contextlib.ExitStack() as st:
            self.st = st
            S = self.S = Sched(nc, st)
            self._alloc_persistent(st)
            self._load_consts()
            l_first = self.layers[0]
            self._cast("wi", l_first * 768, (l_first + 1) * 768)
            self._cast("wo", l_first * 128, (l_first + 1) * 128)
            xin = self.x_in
            bx_in = Buf()
            for li, l in enumerate(self.layers):
                bx1 = Buf()
                self._phase_mixer(l, xin, bx_in, self.x1[l], bx1)
                if self.stop_after == ("M", l):
                    break
                xout = self.x2 if (li + 1 < len(self.layers)) else self.out
                bxo = Buf()
                self._phase_ffn(l, self.x1[l], bx1, xout, bxo)
                xin, bx_in = xout, bxo
            S.barrier()
            S.emit()
        return nc

    def _alloc_persistent(self, st):
        nc = self.nc
        self.psum = []
        self.psb = []
        for i in range(4):
            t = st.enter_context(nc.psum_tensor(f"ps{i}", [128, 1024], F32))
            self.psum.append(t)
            self.psb.append((Buf(), Buf()))
        self.ps_h = 0
        self.ps_f = 0
        self.cst = st.enter_context(nc.sbuf_tensor("s_cstf", [128, CSTF_COLS], F32))
        self.cstb = st.enter_context(nc.sbuf_tensor("s_cstb", [128, CSTB_COLS], BF16))
        self.b_cst = Buf()
        self.b_cstb = Buf()

    def ps_resv(self):
        return self.psum[3][:, 512:1024], [self.psb[3][1]]

    def _half(self, i):
        t = self.psum[i // 2]
        lo = (i % 2) * 512
        return t[:, lo:lo + 512], [self.psb[i // 2][i % 2]]

    def ps_half(self, pool=None):
        if pool == "fx":
            self._fxh = (getattr(self, "_fxh", 0) + 1) % 3
            return self._half(4 + self._fxh)
        if pool == "dn":
            self._dnh = (getattr(self, "_dnh", 0) + 1) % 4
            return self._half(self._dnh)
        i = self.ps_h
        self.ps_h = (i + 1) % 7
        return self._half(i)

    def ps_full(self, pool=None):
        if pool == "dn":
            self._dnf = (getattr(self, "_dnf", 0) + 1) % 2
            i = self._dnf
            return self.psum[i][:, :], list(self.psb[i])
        i = self.ps_f
        self.ps_f = (i + 1) % 3
        return self.psum[i][:, :], list(self.psb[i])

    def sb(self, st, name, shape, dt):
        self._uid = getattr(self, "_uid", 0) + 1
        return st.enter_context(self.nc.sbuf_tensor(f"s{self._uid}_{name}", list(shape), dt))

    def _cast(self, name, r0=None, r1=None):
        S = self.S
        src, dst = {"wi": (self.d_wi, self.b_wi), "wo": (self.d_wo, self.b_wo), "wgu": (self.d_wgu, self.b_wgu),
                    "wd": (self.d_wd, self.b_wd), "mgu": (self.d_mgu, self.b_mgu), "md": (self.d_md, self.b_md)}[name]
        rows = src.shape[0]
        r0 = 0 if r0 is None else r0
        r1 = rows if r1 is None else r1
        for r in range(r0, r1, 128):
            n = min(128, r1 - r)
            bb = Buf()
            self.wbuf[(name, r)] = bb
            S.dma("pool", dst[r:r + n, :], src[r:r + n, :], writes=[bb])

    def _cast_for_ffn(self, l):
        if l % 2 == 0:
            self._cast("wgu")
            self._cast("wd")
        else:
            if ("mgu", 0) not in self.wbuf:
                self._cast("mgu")
                self._cast("md")

    def _cast_for_next_layer(self, li):
        todo = []
        for l2 in self.layers[li + 1:]:
            if ("wi", l2 * 768) not in self.wbuf:
                todo += [("wi", r, r + 128) for r in range(l2 * 768, (l2 + 1) * 768, 128)]
                todo += [("wo", l2 * 128, (l2 + 1) * 128)]
            if l2 % 2 == 1 and ("mgu", 0) not in self.wbuf:
                todo += [("mgu", r, r + 128) for r in range(0, 48 * 128, 128)]
                todo += [("md", r, r + 128) for r in range(0, 8 * 128, 128)]
        return todo

    def _load_consts(self):
        S = self.S
        S.dma("sp", self.cst[:], self.d_cstf, writes=[self.b_cst])
        S.dma("pool", self.cstb[:], self.d_cstb, writes=[self.b_cstb])
        cst, cstb = self.cst, self.cstb
        self.ident_f = cst[:, 0:128]
        self.tri_f = cst[:, 128:256]
        self.ones_f = cst[:, 256:384]
        self.negstrict = cst[:, 384:512]
        self.pinvw = cst[:, 512:514]
        self.pinvc = [cst[:, 516 + 16 * c:516 + 16 * (c + 1)] for c in range(2)]
        self.ident_b = cstb[:, 0:128]
        self.blk2_b = cstb[:, 128:256]
        self.negM_b = [cstb[:, 256 + 128 * l:256 + 128 * (l + 1)] for l in range(7)]
        self.dmask_b = [cstb[:, 1152 + SBM * m:1152 + SBM * (m + 1)] for m in range(NTB)]
        self.negM0T_b = cstb[:, 1152 + SBM * NTB:1152 + SBM * NTB + 128]
        self.CB = [self.b_cst, self.b_cstb]

    def _norm_T(self, xt, bxt, gT, bg, hT_dst, bhT, work, want_f32=None):
        S = self.S
        sq, bsq, ss, bss, rs, brs, xn, bxn = work
        S.op("act", lambda e: e.activation(out=xn[:], in_=xt, func=AF.Square, accum_out=ss[:]), reads=[bxt], writes=[bxn, bss])
        S.op("act", lambda e: e.activation(out=rs[:], in_=ss[:], func=AF.Sqrt, scale=1.0 / D, bias=EPS), reads=[bss], writes=[brs])
        S.op("dve", lambda e: e.reciprocal(out=rs[:], in_=rs[:]), reads=[brs], writes=[brs])
        S.op("act", lambda e: e.activation(out=xn[:], in_=xt, func=AF.Copy, scale=rs[:, 0:1]), reads=[bxt, brs], writes=[bxn])
        pT, bpT = self.ps_half()
        pTb = pT.bitcast(BF16).rearrange("p (a b) -> p a b", a=8)
        ident_b = self.ident_b

        def tr(e):
            ins = None
            for c in range(8):
                ins = e.transpose(out=pTb[:, c, :], in_=xn[:, c * 128:(c + 1) * 128], identity=ident_b)
            return ins
        S.op("pe", tr, reads=[bxn] + self.CB, writes=bpT)
        S.op("dve", lambda e: e.tensor_tensor(out=hT_dst, in0=pTb, in1=gT.unsqueeze(2).to_broadcast([128, 8, 128]), op=ALU.mult),
             reads=bpT + [bg], writes=[bhT])
        if want_f32 is not None:
            dstf, bdf, xf, bxf = want_f32
            S.op("dve", lambda e: e.tensor_scalar(out=xf[:], in0=xt, scalar1=rs[:, 0:1], scalar2=None, op0=ALU.mult),
                 reads=[bxt, brs], writes=[bxf])
            pF, bpF = self.ps_full()
            pFv = pF.rearrange("p (a b) -> p a b", a=8)
            ident_f = self.ident_f

            def trf(e):
                ins = None
                for c in range(8):
                    ins = e.transpose(out=pFv[:, c, :], in_=xf[:, c * 128:(c + 1) * 128], identity=ident_f)
                return ins
            S.op("pe", trf, reads=[bxf] + self.CB, writes=bpF)
            for c0 in (0, 4):
                S.op("dve", lambda e, c0=c0: e.tensor_tensor(out=dstf[:, c0:c0 + 4, :], in0=pFv[:, c0:c0 + 4, :], in1=gT[:, c0:c0 + 4].unsqueeze(2).to_broadcast([128, 4, 128]), op=ALU.mult),
                     reads=bpF + [bg], writes=[bdf])

    def _phase_mixer(self, l, xin, bxin, x1, bx1):
        nc, S = self.nc, self.S
        CB = self.CB
        with contextlib.ExitStack() as ph:
            sb = lambda name, shape, dt: self.sb(ph, name, shape, dt)
            spt = sb("spt", [128, SP_COLS], F32); bsp = Buf()
            S.dma("sp", spt[:], self.d_sp[l * 128:(l + 1) * 128, :], writes=[bsp])
            g1T = spt[:, 0:8]
            convw = spt[:, 16:52].rearrange("p (c j) -> p c j", c=9)
            alog = spt[:, 52:58]; dtb = spt[:, 58:64]
            onorm = spt[:, 64:128]
            fxqg = spt[:, 128:129]; fxkg = spt[:, 129:130]
            fbias = spt[:, 130:136]
            pscale = spt[:, 136:138]
            drv = sb("drv", [128, 16], F32); bdrv = Buf()
            S.op("act", lambda e: e.activation(out=drv[:, 0:6], in_=alog, func=AF.Exp), reads=[bsp], writes=[bdrv])
            S.op("dve", lambda e: e.tensor_scalar(out=drv[:, 0:6], in0=drv[:, 0:6], scalar1=-1.0, scalar2=None, op0=ALU.mult), reads=[bdrv], writes=[bdrv])
            S.op("dve", lambda e: e.tensor_scalar(out=drv[:, 8:9], in0=fxqg, scalar1=0.125, scalar2=None, op0=ALU.mult), reads=[bsp, bdrv], writes=[bdrv])
            aneg = drv[:, 0:6]; fxqg8 = drv[:, 8:9]
            pwf = sb("pwf", [128, 256], F32); bpwf = Buf()
            pwb = sb("pwb", [128, 2, 128], BF16); bpwb = Buf()
            S.dma("sp", pwf[:], self.d_pw[l * 128:(l + 1) * 128, :], writes=[bpwf])
            S.op("dve", lambda e: e.tensor_copy(out=pwb[:].rearrange("p a b -> p (a b)"), in_=pwf[:]), reads=[bpwf], writes=[bpwb])
            wo = sb("wo", [128, 8, 1024], BF16); bwo = Buf()
            S.dma("sp", wo[:].rearrange("p a b -> p (a b)"), self.b_wo[l * 128:(l + 1) * 128, :], reads=[self.wbuf[("wo", l * 128)]], writes=[bwo])
            KT = sb("KT", [128, 3, T], BF16); bKT = Buf()
            V1 = sb("V1", [128, 32, 6, 65], BF16); bV1 = Buf()
            S.op("pool", lambda e: e.memset(V1[:].rearrange("p a b c -> p (a b c)"), 1.0), writes=[bV1])
            c_all = sb("c_all", [128, 32, 6], F32); bcall = Buf()
            carry = sb("carry", [128, 6], F32); bcarry = Buf()
            S.op("pool", lambda e: e.memset(carry[:], 0.0), writes=[bcarry])
            dhalo = sb("dhalo", [128, 9, 3], F32); bdhalo = Buf()
            S.op("pool", lambda e: e.memset(dhalo[:].rearrange("p a b -> p (a b)"), 0.0), writes=[bdhalo])
            phalo = sb("phalo", [128, 2, 16], F32); bphalo = Buf()
            S.op("pool", lambda e: e.memset(phalo[:].rearrange("p a b -> p (a b)"), 0.0), writes=[bphalo])
            S32 = sb("S32", [64, 6, 64], F32); bS32 = Buf()
            Sb = sb("Sb", [64, 6, 64], BF16); bSb = Buf()
            S.op("pool", lambda e: e.memset(S32[:].rearrange("p a b -> p (a b)"), 0.0), writes=[bS32])
            S.op("pool", lambda e: e.memset(Sb[:].rearrange("p a b -> p (a b)"), 0.0), writes=[bSb])
            id6 = sb("id6", [128, 6, 128], BF16); bid6 = Buf()
            S.op("dve", lambda e: e.tensor_copy(out=id6[:], in_=self.ident_b.unsqueeze(1).to_broadcast([128, 6, 128])), reads=CB, writes=[bid6])
            xts = [(sb(f"xt{i}", [128, D], F32), Buf()) for i in range(2)]
            nwork = (None, None, sb("n_ss", [128, 1], F32), Buf(), sb("n_rs", [128, 1], F32), Buf(),
                     sb("n_xn", [128, D], BF16), Buf())
            hT = sb("hT", [128, 8, SBM], BF16); bhT = Buf()
            wis = [(sb(f"wi{i}", [128, 8, 512], BF16), Buf()) for i in range(2)]
            cin = [(sb(f"cin{i}", [128, 3 + SBM], F32), Buf()) for i in range(2)]
            cacc = [(sb(f"cacc{i}", [128, SBM], F32), Buf()) for i in range(2)]
            sqb = [(sb(f"sqb{i}", [128, SBM], BF16), Buf()) for i in range(2)]
            rsd = [(sb(f"rsd{i}", [128, SBM], F32), Buf()) for i in range(2)]
            dnT = sb("dnT", [128, 9, SBM], BF16); bdnT = [Buf() for _ in range(9)]
            qal = sb("qal", [64, 6, SBM], BF16); bqal = Buf()
            kal = sb("kal", [64, 6, SBM], BF16); bkal = Buf()
            fxq = sb("fxq", [128, 3, SBM], BF16); bfxq = [Buf() for _ in range(3)]
            ktok = sb("ktok", [128, NTB, 384], BF16); bktok = Buf()
            vtok = sb("vtok", [128, NTB, 384], BF16); bvtok = Buf()
            ztok = sb("ztok", [128, NTB, 384], F32); bztok = Buf()
            smt = sb("smt", [128, NTB, 18], F32); bsmt = Buf()
            pin = [(sb(f"pin{i}", [128, 16 + SBM], F32), Buf()) for i in range(2)]
            mixT = sb("mixT", [128, 8, SBM], BF16); bmix = [Buf() for _ in range(8)]
            x1t = [(sb(f"x1t{i}", [128, D], F32), Buf()) for i in range(2)]
            sm = {k: (sb("sm_" + k, [128, NTB, 6], F32), Buf()) for k in
                  ("g", "beta", "logf", "t", "G", "Gl", "eG", "ed", "lb", "cum", "tot", "cref")}
            bias_tab = sb("bias_tab", [128, 32, 6], F32); bbias = Buf()
            W = {}
            for k, shape, dt in (("diff", [128, 6, 128], F32), ("es", [128, 6, 128], BF16), ("ec", [128, 6, 128], BF16),
                                 ("Bp", [128, 6, 128], BF16), ("Ap", [128, 6, 128], BF16),
                                 ("qkT", [128, 6, 128], BF16), ("Xn", [128, 6, 128], BF16),
                                 ("Ua", [128, 6, 128], BF16), ("Ub", [128, 6, 128], BF16),
                                 ("Ta", [128, 6, 128], BF16), ("Tb", [128, 6, 128], BF16),
                                 ("kg", [128, 6, 64], BF16), ("kd", [128, 6, 64], BF16),
                                 ("ut", [128, 6, 64], F32), ("wT", [64, 6, 128], BF16),
                                 ("vn", [128, 6, 64], BF16),
                                 ("o", [128, 6, 64], F32), ("osq", [128, 6, 64], F32), ("oss", [128, 6], F32),
                                 ("ors", [128, 6], F32), ("zs", [128, 384], F32), ("of", [128, 384], BF16)):
                W[k] = (sb("w_" + k, shape, dt), Buf())
            Es = [(sb(f"E{i}", [128, SBM], BF16), Buf()) for i in range(3)]
            rr = sb("rr", [128, SBM], F32); brr = Buf()
            ps2 = sb("ps2", [128, 16 + SBM], F32); bps2 = Buf()
            ps4 = sb("ps4", [128, 16 + SBM], F32); bps4 = Buf()
            ps8 = sb("ps8", [128, 16 + SBM], F32); bps8 = Buf()
            pW = sb("pW", [128, SBM], F32); bpW = Buf()
            py = sb("py", [128, SBM], BF16); bpy = Buf()
            ident_b = self.ident_b

            _st = set(os.environ.get('MK_STAGES', 'C,D,E,F,G').split(','))
            for I in range(min(NSBM, int(os.environ.get('MK_NSB', NSBM)))):
                t0 = I * SBM
                for tb in range(NTB):
                    xt, bxt = xts[tb % 2]
                    S.dma("sp", xt[:], xin[t0 + tb * 128:t0 + (tb + 1) * 128, :], reads=[bxin], writes=[bxt])
                    self._norm_T(xt[:], bxt, g1T, bsp, hT[:, :, tb * 128:(tb + 1) * 128], bhT, nwork)
                pend = None
                for t in range(6):
                    wi, bwi = wis[t % 2]
                    row = (l * 6 + t) * 128
                    S.dma("sp", wi[:].rearrange("p a b -> p (a b)"), self.b_wi[row:row + 128, :], reads=[self.wbuf[("wi", row)]], writes=[bwi])
                    nfm = 4 if t < 4 else (1 if t == 4 else 0)
                    for j in range(nfm):
                        fc = 4 * t + j
                        pp_, bpp = self.ps_half()
                        pp = pp_[:, 0:SBM]

                        def mm(e, wi=wi, j=j, pp=pp):
                            ins = None
                            for kc in range(8):
                                ins = e.matmul(out=pp, lhsT=wi[:, kc, j * 128:(j + 1) * 128], rhs=hT[:, kc, :], start=(kc == 0), stop=(kc == 7))
                            return ins
                        S.op("pe", mm, reads=[bwi, bhT], writes=bpp)
                        if fc < 9:
                            gen = self._dn_conv(fc, pp, bpp, cin[fc % 2], cacc[fc % 2], sqb[fc % 2], rsd[fc % 2], dhalo, bdhalo,
                                                convw, bsp, dnT, bdnT)
                        elif fc < 15:
                            gen = self._fx_qk(fc - 9, I, pp, bpp, sqb[fc % 2], rsd[fc % 2], fxqg8, bdrv, fxkg, bsp, fxq, bfxq, KT, bKT)
                        else:
                            gen = self._pool_mix(fc - 15, I, pp, bpp, pin[fc % 2], phalo, bphalo, ps2, bps2, ps4, bps4, ps8, bps8,
                                                 pW, bpW, py, bpy, pwb, bpwb, pscale, bsp, mixT, bmix)
                        alive = next(gen, "done") != "done"
                        if pend is not None:
                            for _ in pend:
                                pass
                        pend = gen if alive else None
                    if t == 4:
                        for tb in range(NTB):
                            pp, bpp = self.ps_half()

                            def mm(e, wi=wi, tb=tb, pp=pp):
                                ins = None
                                for kc in range(8):
                                    ins = e.matmul(out=pp[:, 0:384], lhsT=hT[:, kc, tb * 128:(tb + 1) * 128], rhs=wi[:, kc, 128:512], start=(kc == 0), stop=(kc == 7))
                                return ins
                            S.op("pe", mm, reads=[bwi, bhT], writes=bpp)
                            blk = NTB * I + tb
                            S.op("act", lambda e, pp=pp, blk=blk: e.activation(out=V1[:, blk, :, 0:64], in_=pp[:, 0:384].rearrange("p (h d) -> p h d", h=6), func=AF.Copy),
                                 reads=bpp, writes=[bV1])
                    if t == 5:
                        for tb in range(NTB):
                            pp, bpp = self.ps_half()

                            def mm(e, wi=wi, tb=tb, pp=pp):
                                ins = None
                                for kc in range(8):
                                    ins = e.matmul(out=pp, lhsT=hT[:, kc, tb * 128:(tb + 1) * 128], rhs=wi[:, kc, :], start=(kc == 0), stop=(kc == 7))
                                return ins
                            S.op("pe", mm, reads=[bwi, bhT], writes=bpp)
                            S.op("act", lambda e, pp=pp, tb=tb: e.activation(out=ztok[:, tb, :], in_=pp[:, 0:384], func=AF.Copy), reads=bpp, writes=[bztok])
                            S.op("dve", lambda e, pp=pp, tb=tb: e.tensor_copy(out=smt[:, tb, :], in_=pp[:, 384:402]), reads=bpp, writes=[bsmt])
                if pend is not None:
                    for _ in pend:
                        pass
                    pend = None
                if 'C' in _st:
                  self._scalars(I, smt, bsmt, sm, aneg, bdrv, dtb, fbias, bsp, c_all, bcall, carry, bcarry, bias_tab, bbias)
                if 'D' not in _st:
                    continue
                S.op("dve", lambda e: e.tensor_copy(out=qal[:, 0:3, :], in_=dnT[0:64, 0:3, :]), reads=bdnT[0:3], writes=[bqal])
                S.op("dve", lambda e: e.tensor_copy(out=qal[:, 3:6, :], in_=dnT[64:128, 0:3, :]), reads=bdnT[0:3] + [bqal], writes=[bqal])
                S.op("dve", lambda e: e.tensor_copy(out=kal[:, 0:3, :], in_=dnT[0:64, 3:6, :]), reads=bdnT[3:6], writes=[bkal])
                S.op("dve", lambda e: e.tensor_copy(out=kal[:, 3:6, :], in_=dnT[64:128, 3:6, :]), reads=bdnT[3:6] + [bkal], writes=[bkal])
                for tb in range(NTB):
                    for which, dst, bdst in ((3, ktok, bktok), (6, vtok, bvtok)):
                        pT, bpT = self.ps_half()
                        pTb = pT.bitcast(BF16)

                        def tr(e, tb=tb, which=which, pTb=pTb):
                            ins = None
                            for c in range(3):
                                ins = e.transpose(out=pTb[:, c * 128:(c + 1) * 128], in_=dnT[:, which + c, tb * 128:(tb + 1) * 128], identity=ident_b)
                            return ins
                        S.op("pe", tr, reads=[bdnT[which], bdnT[which + 1], bdnT[which + 2]] + CB, writes=bpT)
                        S.op("act", lambda e, dst=dst, tb=tb, pTb=pTb: e.activation(out=dst[:, tb, :], in_=pTb[:, 0:384], func=AF.Copy), reads=bpT, writes=[bdst])
                def dn_all(I=I):
                    for tb in range(NTB if 'E' in _st else 0):
                        yield from self._dn_chunk(I, tb, dnT, bdnT, qal, bqal, kal, bkal, ktok, bktok, vtok, bvtok, ztok, bztok, sm, W, S32, bS32, Sb, bSb, id6, bid6,
                                                  onorm, bsp, mixT, bmix)

                def fx_all(I=I):
                    for h in range(6 if 'F' in _st else 0):
                        yield from self._fox_head(I, h, fxq, bfxq, KT, bKT, V1, bV1, bias_tab, bbias, Es, rr, brr, mixT, bmix)
                n_dn = NTB * 26
                n_fx = 6 * (NTB * I + NTB + 1)
                g1, g2 = dn_all(), fx_all()
                d1 = d2 = 0
                a1 = a2 = True
                while a1 or a2:
                    if a1 and (not a2 or d1 * n_fx <= d2 * n_dn):
                        try:
                            next(g1); d1 += 1
                        except StopIteration:
                            a1 = False
                    else:
                        try:
                            next(g2); d2 += 1
                        except StopIteration:
                            a2 = False
                for tb in range(NTB if 'G' in _st else 0):
                    xt, bxt = xts[tb % 2]
                    S.dma("sp", xt[:], xin[t0 + tb * 128:t0 + (tb + 1) * 128, :], reads=[bxin], writes=[bxt])
                    xo, bxo = x1t[tb % 2]
                    for nh in range(2):
                        pp, bpp = self.ps_half()

                        def mm(e, tb=tb, nh=nh, pp=pp):
                            ins = None
                            for c in range(8):
                                ins = e.matmul(out=pp, lhsT=mixT[:, c, tb * 128:(tb + 1) * 128], rhs=wo[:, c, nh * 512:(nh + 1) * 512], start=(c == 0), stop=(c == 7))
                            return ins
                        S.op("pe", mm, reads=bmix + [bwo], writes=bpp)
                        S.op("dve", lambda e, xo=xo, xt=xt, nh=nh, pp=pp: e.tensor_tensor(out=xo[:, nh * 512:(nh + 1) * 512], in0=pp, in1=xt[:, nh * 512:(nh + 1) * 512], op=ALU.add),
                             reads=bpp + [bxt], writes=[bxo])
                    S.dma("pool", x1[t0 + tb * 128:t0 + (tb + 1) * 128, :], xo[:], reads=[bxo], writes=[bx1], add=True)
                if I == 0:
                    self._cast_for_ffn(l)
            S.barrier()
            with nc.named_scope(f"mixer{l}"):
                S.emit()

    def _dn_conv(self, fc, pp, bpp, cin_, cacc_, sqb_, rsd_, dhalo, bdhalo, convw, bsp, dnT, bdnT):
        S = self.S
        cin, bcin = cin_; acc, bacc = cacc_; sq, bsq = sqb_; rs, brs = rsd_
        S.op("act", lambda e: e.activation(out=cin[:, 3:3 + SBM], in_=pp, func=AF.Copy), reads=bpp, writes=[bcin])
        S.op("pool", lambda e: e.tensor_copy(out=cin[:, 0:3], in_=dhalo[:, fc, :]), reads=[bdhalo, bcin], writes=[bcin])
        S.op("pool", lambda e: e.tensor_copy(out=dhalo[:, fc, :], in_=cin[:, SBM:SBM + 3]), reads=[bcin], writes=[bdhalo])
        S.op("dve", lambda e: e.tensor_scalar(out=acc[:], in0=cin[:, 3:3 + SBM], scalar1=convw[:, fc, 3:4], scalar2=None, op0=ALU.mult),
             reads=[bcin, bsp], writes=[bacc])
        for j in range(3):
            eng = "dve"
            S.op(eng, lambda e, j=j: e.scalar_tensor_tensor(out=acc[:], in0=cin[:, j:j + SBM], scalar=convw[:, fc, j:j + 1], in1=acc[:], op0=ALU.mult, op1=ALU.add),
                 reads=[bcin, bsp, bacc], writes=[bacc])
        if fc >= 6:
            S.op("act", lambda e: e.activation(out=dnT[:, fc, :], in_=acc[:], func=AF.Silu), reads=[bacc], writes=[bdnT[fc]])
            return
        S.op("act", lambda e: e.activation(out=acc[:], in_=acc[:], func=AF.Silu), reads=[bacc], writes=[bacc])
        S.op("pool", lambda e: e.tensor_tensor(out=sq[:], in0=acc[:], in1=acc[:], op=ALU.mult), reads=[bacc], writes=[bsq])
        p2_, bp2 = self.ps_half()
        p2 = p2_[:, 0:SBM]
        blk2_b = self.blk2_b
        S.op("pe", lambda e: e.matmul(out=p2, lhsT=blk2_b, rhs=sq[:], start=True, stop=True), reads=[bsq] + self.CB, writes=bp2)
        yield
        S.op("act", lambda e: e.activation(out=rs[:], in_=p2, func=AF.Sqrt, bias=EPS), reads=bp2, writes=[brs])
        S.op("dve", lambda e: e.reciprocal(out=rs[:], in_=rs[:]), reads=[brs], writes=[brs])
        scl = 0.125 if fc < 3 else 1.0
        S.op("dve", lambda e: e.scalar_tensor_tensor(out=dnT[:, fc, :], in0=acc[:], scalar=scl, in1=rs[:], op0=ALU.mult, op1=ALU.mult),
             reads=[bacc, brs], writes=[bdnT[fc]])

    def _fx_qk(self, c, I, pp, bpp, sqb_, rsd_, fxqg8, bdrv, fxkg, bsp, fxq, bfxq, KT, bKT):
        S = self.S
        sq, bsq = sqb_; rs, brs = rsd_
        S.op("act", lambda e: e.activation(out=sq[:], in_=pp, func=AF.Square), reads=bpp, writes=[bsq])
        p2_, bp2 = self.ps_half()
        p2 = p2_[:, 0:SBM]
        blk2_b = self.blk2_b
        S.op("pe", lambda e: e.matmul(out=p2, lhsT=blk2_b, rhs=sq[:], start=True, stop=True), reads=[bsq] + self.CB, writes=bp2)
        yield
        S.op("act", lambda e: e.activation(out=rs[:], in_=p2, func=AF.Sqrt, scale=1.0 / 64, bias=EPS), reads=bp2, writes=[brs])
        S.op("dve", lambda e: e.reciprocal(out=rs[:], in_=rs[:]), reads=[brs], writes=[brs])
        if c < 3:
            S.op("dve", lambda e: e.scalar_tensor_tensor(out=fxq[:, c, :], in0=pp, scalar=fxqg8, in1=rs[:], op0=ALU.mult, op1=ALU.mult),
                 reads=bpp + [bdrv, brs], writes=[bfxq[c]])
        else:
            S.op("dve", lambda e: e.scalar_tensor_tensor(out=KT[:, c - 3, I * SBM:(I + 1) * SBM], in0=pp, scalar=fxkg, in1=rs[:], op0=ALU.mult, op1=ALU.mult),
                 reads=bpp + [bsp, brs], writes=[bKT])

    def _pool_mix(self, c, I, pp, bpp, pin_, phalo, bphalo, ps2, bps2, ps4, bps4, ps8, bps8, pW, bpW, py, bpy, pwb, bpwb, pscale, bsp, mixT, bmix):
        S = self.S
        pin, bpin = pin_
        N = 16 + SBM
        S.op("act", lambda e: e.activation(out=pin[:, 16:N], in_=pp, func=AF.Copy), reads=bpp, writes=[bpin])
        S.op("pool", lambda e: e.tensor_copy(out=pin[:, 0:16], in_=phalo[:, c, :]), reads=[bphalo, bpin], writes=[bpin])
        S.op("pool", lambda e: e.tensor_copy(out=phalo[:, c, :], in_=pin[:, SBM:N]), reads=[bpin], writes=[bphalo])
        S.op("pool", lambda e: e.tensor_tensor(out=ps2[:, 2:N], in0=pin[:, 2:N], in1=pin[:, 1:N - 1], op=ALU.add), reads=[bpin], writes=[bps2])
        if c == 0:
            S.op("pool", lambda e: e.tensor_copy(out=pW[0:64, :], in_=ps2[0:64, 16:N]), reads=[bps2], writes=[bpW])
            S.op("pool", lambda e: e.tensor_tensor(out=pW[64:128, :], in0=ps2[64:128, 16:N], in1=ps2[64:128, 14:N - 2], op=ALU.add), reads=[bps2, bpW], writes=[bpW])
        else:
            S.op("pool", lambda e: e.tensor_tensor(out=ps4[:, 4:N], in0=ps2[:, 4:N], in1=ps2[:, 2:N - 2], op=ALU.add), reads=[bps2], writes=[bps4])
            S.op("pool", lambda e: e.tensor_tensor(out=ps8[:, 8:N], in0=ps4[:, 8:N], in1=ps4[:, 4:N - 4], op=ALU.add), reads=[bps4], writes=[bps8])
            S.op("pool", lambda e: e.tensor_copy(out=pW[0:64, :], in_=ps8[0:64, 16:N]), reads=[bps8], writes=[bpW])
            S.op("pool", lambda e: e.tensor_tensor(out=pW[64:128, :], in0=ps8[64:128, 16:N], in1=ps8[64:128, 8:N - 8], op=ALU.add), reads=[bps8, bpW], writes=[bpW])
        pinvw = self.pinvw
        S.op("dve", lambda e: e.scalar_tensor_tensor(out=py[:], in0=pW[:], scalar=pinvw[:, c:c + 1], in1=pin[:, 16:N], op0=ALU.mult, op1=ALU.subtract),
             reads=[bpW, bpin] + self.CB, writes=[bpy])
        if I == 0:
            pinvc = self.pinvc[c]
            S.op("dve", lambda e: e.tensor_tensor(out=pW[:, 0:16], in0=pW[:, 0:16], in1=pinvc, op=ALU.mult), reads=[bpW, bpy] + self.CB, writes=[bpW])
            S.op("dve", lambda e: e.tensor_tensor(out=py[:, 0:16], in0=pW[:, 0:16], in1=pin[:, 16:32], op=ALU.subtract), reads=[bpW, bpin, bpy], writes=[bpy])
        p2_, bp2 = self.ps_half()
        p2 = p2_[:, 0:SBM]
        S.op("pe", lambda e: e.matmul(out=p2, lhsT=pwb[:, c, :], rhs=py[:], start=True, stop=True), reads=[bpy, bpwb], writes=bp2)
        S.op("act", lambda e: e.activation(out=mixT[:, 6 + c, :], in_=p2, func=AF.Copy, scale=pscale[:, c:c + 1]), reads=bp2 + [bsp], writes=[bmix[6 + c]])
        return
        yield

    def _scalars(self, I, smt, bsmt, sm, aneg, bdrv, dtb, fbias, bsp, c_all, bcall, carry, bcarry, bias_tab, bbias):
        S = self.S
        g, bg = sm["g"]; beta, bbeta = sm["beta"]; logf, blogf = sm["logf"]; tt, btt = sm["t"]
        G, bG = sm["G"]; Gl, bGl = sm["Gl"]; eG, beG = sm["eG"]; ed, bed = sm["ed"]; lb, blb = sm["lb"]
        cum, bcum = sm["cum"]; tot, btot = sm["tot"]; cref, bcref = sm["cref"]
        NC = NTB * 6
        bc4 = lambda ap: ap.unsqueeze(1).to_broadcast([128, NTB, 6])
        S.op("dve", lambda e: e.tensor_tensor(out=tt[:], in0=smt[:, :, 0:6], in1=bc4(dtb), op=ALU.add), reads=[bsmt, bsp], writes=[btt])
        S.op("act", lambda e: e.activation(out=tt[:], in_=tt[:], func=AF.Exp), reads=[btt], writes=[btt])
        S.op("act", lambda e: e.activation(out=tt[:], in_=tt[:], func=AF.Ln, bias=1.0), reads=[btt], writes=[btt])
        S.op("dve", lambda e: e.tensor_tensor(out=g[:], in0=tt[:], in1=bc4(aneg), op=ALU.mult), reads=[btt, bdrv], writes=[bg])
        S.op("dve", lambda e: e.tensor_tensor(out=tt[:], in0=smt[:, :, 12:18], in1=bc4(fbias), op=ALU.add), reads=[bsmt, bsp, btt], writes=[btt])
        S.op("act", lambda e: e.activation(out=tt[:], in_=tt[:], func=AF.Exp, scale=-1.0), reads=[btt], writes=[btt])
        S.op("act", lambda e: e.activation(out=tt[:], in_=tt[:], func=AF.Ln, bias=1.0), reads=[btt], writes=[btt])
        S.op("dve", lambda e: e.tensor_scalar(out=logf[:], in0=tt[:], scalar1=-1.0, scalar2=None, op0=ALU.mult), reads=[btt], writes=[blogf])
        S.op("act", lambda e: e.activation(out=beta[:], in_=smt[:, :, 6:12], func=AF.Sigmoid), reads=[bsmt], writes=[bbeta])
        tri_f, ones_f = self.tri_f, self.ones_f
        fl = lambda t: t[:].rearrange("p a b -> p (a b)")
        p1, bp1 = self.ps_half()
        S.op("pe", lambda e: e.matmul(out=p1[:, 0:NC], lhsT=tri_f, rhs=fl(g), start=True, stop=True), reads=[bg] + self.CB, writes=bp1)
        S.op("dve", lambda e: e.tensor_copy(out=fl(G), in_=p1[:, 0:NC]), reads=bp1, writes=[bG])
        p2, bp2 = self.ps_half()
        S.op("pe", lambda e: e.matmul(out=p2[:, 0:NC], lhsT=ones_f, rhs=fl(g), start=True, stop=True), reads=[bg] + self.CB, writes=bp2)
        S.op("dve", lambda e: e.tensor_copy(out=fl(Gl), in_=p2[:, 0:NC]), reads=bp2, writes=[bGl])
        S.op("act", lambda e: e.activation(out=eG[:], in_=G[:], func=AF.Exp), reads=[bG], writes=[beG])
        S.op("act", lambda e: e.activation(out=lb[:], in_=Gl[:], func=AF.Exp), reads=[bGl], writes=[blb])
        S.op("dve", lambda e: e.tensor_tensor(out=ed[:], in0=Gl[:], in1=G[:], op=ALU.subtract), reads=[bGl, bG], writes=[bed])
        S.op("act", lambda e: e.activation(out=ed[:], in_=ed[:], func=AF.Exp), reads=[bed], writes=[bed])
        p3, bp3 = self.ps_half()
        S.op("pe", lambda e: e.matmul(out=p3[:, 0:NC], lhsT=tri_f, rhs=fl(logf), start=True, stop=True), reads=[blogf] + self.CB, writes=bp3)
        S.op("dve", lambda e: e.tensor_copy(out=fl(cum), in_=p3[:, 0:NC]), reads=bp3, writes=[bcum])
        p4, bp4 = self.ps_half()
        S.op("pe", lambda e: e.matmul(out=p4[:, 0:NC], lhsT=ones_f, rhs=fl(logf), start=True, stop=True), reads=[blogf] + self.CB, writes=bp4)
        S.op("dve", lambda e: e.tensor_copy(out=fl(tot), in_=p4[:, 0:NC]), reads=bp4, writes=[btot])
        for tb in range(NTB):
            blk = NTB * I + tb
            S.op("dve", lambda e, tb=tb, blk=blk: e.tensor_tensor(out=c_all[:, blk, :], in0=cum[:, tb, :], in1=carry[:], op=ALU.add),
                 reads=[bcum, bcarry], writes=[bcall])
            S.op("dve", lambda e, tb=tb: e.tensor_tensor(out=carry[:], in0=carry[:], in1=tot[:, tb, :], op=ALU.add),
                 reads=[btot, bcarry], writes=[bcarry])
            if tb == NTB // 2 - 1:
                S.op("dve", lambda e: e.tensor_copy(out=cref[:, 0, :], in_=carry[:]), reads=[bcarry], writes=[bcref])
        nb = NTB * I + NTB
        S.op("dve", lambda e: e.tensor_tensor(out=bias_tab[:, 0:nb, :], in0=cref[:, 0, :].unsqueeze(1).to_broadcast([128, nb, 6]), in1=c_all[:, 0:nb, :], op=ALU.subtract),
             reads=[bcref, bcall], writes=[bbias])

    def _dn_chunk(self, I, tb, dnT, bdnT, qal, bqal, kal, bkal, ktok, bktok, vtok, bvtok, ztok, bztok, sm, W, S32, bS32, Sb, bSb, id6, bid6, onorm, bsp, mixT, bmix):
        S = self.S
        CB = self.CB
        g, bg = sm["g"]; beta, bbeta = sm["beta"]; G, bG = sm["G"]; eG, beG = sm["eG"]; ed, bed = sm["ed"]; lb, blb = sm["lb"]
        ts = slice(tb * 128, (tb + 1) * 128)
        hs = lambda h: slice((h % 2) * 64, (h % 2) * 64 + 64)
        qT = lambda h: dnT[hs(h), h // 2, ts]
        kT = lambda h: dnT[hs(h), 3 + h // 2, ts]
        bq = bdnT[0:3]; bk = bdnT[3:6]
        v6 = lambda ap: ap.rearrange("p (h d) -> p h d", h=6)
        bc128 = lambda ap: ap.unsqueeze(2).to_broadcast([128, 6, 128])
        bc64 = lambda ap: ap.unsqueeze(2).to_broadcast([128, 6, 64])
        tri_f, ident_b, ident_f = self.tri_f, self.ident_b, self.ident_f
        diff, bdiff = W["diff"]; es, bes = W["es"]; ec, bec = W["ec"]
        Bp, bBp = W["Bp"]; Ap, bAp = W["Ap"]; qkT, bqkT = W["qkT"]; Xn, bXn = W["Xn"]
        kg, bkg = W["kg"]; kd, bkd = W["kd"]; ut, but = W["ut"]; wT, bwT = W["wT"]
        vn, bvn = W["vn"]; o, bo = W["o"]; osq, bosq = W["osq"]; oss, boss = W["oss"]; ors, bors = W["ors"]
        zs, bzs = W["zs"]; of, bof = W["of"]
        HS = (slice(0, 4), slice(4, 6))

        def two(eng, fn, reads, writes):
            S.op(eng, lambda e: fn(e, slice(0, 6)), reads=reads, writes=writes)
        bc128s = lambda ap, hsl: ap[:, hsl].unsqueeze(2).to_broadcast([128, hsl.stop - hsl.start, 128])
        mk6 = lambda m, hsl: m.unsqueeze(1).to_broadcast([128, hsl.stop - hsl.start, 128])
        pG, bpG = self.ps_full("dn")
        pG6 = pG[:, 0:768].rearrange("p (h i) -> p h i", h=6)

        def mmG(e):
            ins = None
            for h in range(6):
                ins = e.matmul(out=pG6[:, h, :], lhsT=g[:, tb, h:h + 1].to_broadcast([128, 128]), rhs=tri_f, start=True, stop=True)
            return ins
        S.op("pe", mmG, reads=[bg] + CB, writes=bpG)
        two("dve", lambda e, hsl: e.tensor_tensor(out=diff[:, hsl, :], in0=pG6[:, hsl, :], in1=bc128s(G[:, tb, :], hsl), op=ALU.subtract), bpG + [bG], [bdiff])
        negstrict = self.negstrict
        S.op("dve", lambda e: e.scalar_tensor_tensor(out=diff[:], in0=diff[:], scalar=0.0, in1=negstrict.unsqueeze(1).to_broadcast([128, 6, 128]), op0=ALU.min, op1=ALU.add),
             reads=[bdiff] + CB, writes=[bdiff])
        S.op("act", lambda e: e.activation(out=es[:], in_=diff[:], func=AF.Exp), reads=[bdiff], writes=[bes])
        S.op("dve", lambda e: e.tensor_tensor(out=ec[:], in0=es[:], in1=ident_b.unsqueeze(1).to_broadcast([128, 6, 128]), op=ALU.add), reads=[bes] + CB, writes=[bec])
        if int(os.environ.get('MK_E1', 9)) < 2:
            return
        yield
        pK, bpK = self.ps_full("dn")
        pK6 = pK[:, 0:768].rearrange("p (h i) -> p h i", h=6)

        def mmK(e):
            ins = None
            for h in range(6):
                ins = e.matmul(out=pK6[:, h, :], lhsT=kal[:, (h % 2) * 3 + h // 2, ts], rhs=kal[:, (h % 2) * 3 + h // 2, ts], start=True, stop=True)
            return ins
        S.op("pe", mmK, reads=[bkal], writes=bpK)
        two("dve", lambda e, hsl: e.tensor_tensor(out=diff[:, hsl, :], in0=pK6[:, hsl, :], in1=es[:, hsl, :], op=ALU.mult), bpK + [bes, bdiff], [bdiff])
        S.op("dve", lambda e: e.tensor_tensor(out=Bp[:], in0=diff[:], in1=bc128(beta[:, tb, :]), op=ALU.mult), reads=[bdiff, bbeta], writes=[bBp])
        yield
        pQ, bpQ = self.ps_full("dn")
        pQ6 = pQ[:, 0:768].rearrange("p (h i) -> p h i", h=6)

        def mmQ(e):
            ins = None
            for h in range(6):
                ins = e.matmul(out=pQ6[:, h, :], lhsT=kal[:, (h % 2) * 3 + h // 2, ts], rhs=qal[:, (h % 2) * 3 + h // 2, ts], start=True, stop=True)
            return ins
        S.op("pe", mmQ, reads=[bkal, bqal], writes=bpQ)
        two("dve", lambda e, hsl: e.tensor_tensor(out=qkT[:, hsl, :], in0=pQ6[:, hsl, :], in1=ec[:, hsl, :], op=ALU.mult), bpQ + [bec], [bqkT])
        if int(os.environ.get('MK_E1', 9)) < 3:
            return
        yield
        pA, bpA = self.ps_half("dn")
        pA6 = pA.bitcast(BF16)[:, 0:768].rearrange("p (h i) -> p h i", h=6)

        def trA(e):
            ins = None
            for h in range(6):
                ins = e.transpose(out=pA6[:, h, :], in_=Bp[:, h, :], identity=ident_b)
            return ins
        S.op("pe", trA, reads=[bBp] + CB, writes=bpA)
        S.op("act", lambda e: e.activation(out=Ap[:], in_=pA6, func=AF.Copy), reads=bpA, writes=[bAp])
        if int(os.environ.get('MK_E', 9)) < 2:
            return
        yield
        U, bU = id6, bid6
        Tm, bT = id6, bid6
        Us = [W["Ua"], W["Ub"]]; Ts = [W["Ta"], W["Tb"]]
        negM0, negM0T = self.negM_b[0], self.negM0T_b
        Ua0, bUa0 = Us[0]
        Ta0, bTa0 = Ts[0]
        S.op("dve", lambda e: e.tensor_tensor(out=Xn[:], in0=Bp[:], in1=negM0.unsqueeze(1).to_broadcast([128, 6, 128]), op=ALU.mult), reads=[bBp] + CB, writes=[bXn])
        S.op("pool", lambda e: e.tensor_tensor(out=Ua0[:], in0=Xn[:], in1=id6[:], op=ALU.add), reads=[bXn, bid6], writes=[bUa0])
        S.op("dve", lambda e: e.tensor_tensor(out=Ta0[:], in0=Ap[:], in1=negM0T.unsqueeze(1).to_broadcast([128, 6, 128]), op=ALU.mult), reads=[bAp] + CB, writes=[bTa0])
        S.op("pool", lambda e: e.tensor_tensor(out=Ta0[:], in0=Ta0[:], in1=id6[:], op=ALU.add), reads=[bTa0, bid6], writes=[bTa0])
        U, bU = Ua0, bUa0
        Tm, bT = Ta0, bTa0
        yield
        for lv in range(1, 7):
            pX, bpX = self.ps_full("dn")
            pX6 = pX[:, 0:768].rearrange("p (h i) -> p h i", h=6)

            def mmX(e, U=U, pX6=pX6):
                ins = None
                for h in range(6):
                    ins = e.matmul(out=pX6[:, h, :], lhsT=Ap[:, h, :], rhs=U[:, h, :], start=True, stop=True)
                return ins
            S.op("pe", mmX, reads=[bAp, bU], writes=bpX)
            negM = self.negM_b[lv]
            two("dve", lambda e, hsl, pX6=pX6, negM=negM: e.tensor_tensor(out=Xn[:, hsl, :], in0=pX6[:, hsl, :], in1=mk6(negM, hsl), op=ALU.mult),
                bpX + CB, [bXn])
            yield
            Un, bUn = Us[lv % 2]
            pU, bpU = self.ps_full("dn")
            pU6 = pU[:, 0:768].rearrange("p (h i) -> p h i", h=6)

            def mmU(e, U=U, Tm=Tm, pU6=pU6):
                ins = None
                for h in range(6):
                    e.matmul(out=pU6[:, h, :], lhsT=ident_b, rhs=U[:, h, :], start=True, stop=False)
                    ins = e.matmul(out=pU6[:, h, :], lhsT=Tm[:, h, :], rhs=Xn[:, h, :], start=False, stop=True)
                return ins
            S.op("pe", mmU, reads=[bU, bT, bXn] + CB, writes=bpU)
            two("act", lambda e, hsl, Un=Un, pU6=pU6: e.activation(out=Un[:, hsl, :], in_=pU6[:, hsl, :], func=AF.Copy), bpU, [bUn])
            if lv < 6:
                Tn, bTn = Ts[lv % 2]
                pT, bpT = self.ps_full("dn")
                pT6 = pT[:, 0:768].rearrange("p (h i) -> p h i", h=6)

                def mmT(e, Tm=Tm, pT6=pT6):
                    ins = None
                    for h in range(6):
                        e.matmul(out=pT6[:, h, :], lhsT=ident_b, rhs=Tm[:, h, :], start=True, stop=False)
                        ins = e.matmul(out=pT6[:, h, :], lhsT=Xn[:, h, :], rhs=Tm[:, h, :], start=False, stop=True)
                    return ins
                S.op("pe", mmT, reads=[bT, bXn] + CB, writes=bpT)
                two("dve", lambda e, hsl, Tn=Tn, pT6=pT6: e.tensor_copy(out=Tn[:, hsl, :], in_=pT6[:, hsl, :]), bpT, [bTn])
                Tm, bT = Tn, bTn
            U, bU = Un, bUn
            yield
        if int(os.environ.get('MK_E', 9)) < 3:
            return
        yield
        S.op("dve", lambda e: e.tensor_tensor(out=kg[:], in0=v6(ktok[:, tb, :]), in1=bc64(eG[:, tb, :]), op=ALU.mult), reads=[bktok, beG], writes=[bkg])
        S.op("dve", lambda e: e.tensor_tensor(out=kd[:], in0=v6(ktok[:, tb, :]), in1=bc64(ed[:, tb, :]), op=ALU.mult), reads=[bktok, bed], writes=[bkd])
        pu, bpu = self.ps_half("dn")
        pu6 = pu[:, 0:384].rearrange("p (h d) -> p h d", h=6)

        def mmu(e, U=U):
            ins = None
            for h in range(6):
                ins = e.matmul(out=pu6[:, h, :], lhsT=U[:, h, :], rhs=vtok[:, tb, h * 64:(h + 1) * 64], start=True, stop=True)
            return ins
        S.op("pe", mmu, reads=[bU, bvtok], writes=bpu)
        S.op("act", lambda e: e.activation(out=ut[:], in_=pu6, func=AF.Copy), reads=bpu, writes=[but])
        yield
        pw_, bpw_ = self.ps_full("dn")
        pw6 = pw_[0:64, 0:768].rearrange("p (h i) -> p h i", h=6)

        def mmw(e, U=U):
            ins = None
            for h in range(6):
                ins = e.matmul(out=pw6[:, h, :], lhsT=kg[:, h, :], rhs=U[:, h, :], start=True, stop=True)
            return ins
        S.op("pe", mmw, reads=[bU, bkg], writes=bpw_)
        two("act", lambda e, hsl: e.activation(out=wT[:, hsl, :], in_=pw6[:, hsl, :], func=AF.Copy), bpw_, [bwT])
        yield
        p1, bp1 = self.ps_half("dn")
        p16 = p1[:, 0:384].rearrange("p (h d) -> p h d", h=6)

        def mm1(e):
            ins = None
            for h in range(6):
                ins = e.matmul(out=p16[:, h, :], lhsT=wT[:, h, :], rhs=Sb[:, h, :], start=True, stop=True)
            return ins
        S.op("pe", mm1, reads=[bwT, bSb], writes=bp1)
        S.op("dve", lambda e: e.tensor_tensor(out=ut[:], in0=ut[:], in1=p16, op=ALU.subtract), reads=bp1 + [but], writes=[but])
        S.op("dve", lambda e: e.tensor_tensor(out=vn[:], in0=ut[:], in1=bc64(beta[:, tb, :]), op=ALU.mult), reads=[but, bbeta], writes=[bvn])
        yield
        p2a, bp2a = self.ps_half("dn")
        p2a6 = p2a[:, 0:384].rearrange("p (h d) -> p h d", h=6)

        def mm2a(e):
            ins = None
            for h in range(6):
                ins = e.matmul(out=p2a6[:, h, :], lhsT=qal[:, (h % 2) * 3 + h // 2, ts], rhs=Sb[:, h, :], start=True, stop=True)
            return ins
        S.op("pe", mm2a, reads=[bqal, bSb], writes=bp2a)
        p2b, bp2b = self.ps_half("dn")
        p2b6 = p2b[:, 0:384].rearrange("p (h d) -> p h d", h=6)

        def mm2b(e):
            ins = None
            for h in range(6):
                ins = e.matmul(out=p2b6[:, h, :], lhsT=qkT[:, h, :], rhs=vn[:, h, :], start=True, stop=True)
            return ins
        S.op("pe", mm2b, reads=[bqkT, bvn], writes=bp2b)
        p3, bp3 = self.ps_half("dn")
        p36 = p3[0:64, 0:384].rearrange("p (h d) -> p h d", h=6)

        def mm3(e):
            ins = None
            for h in range(6):
                ins = e.matmul(out=p36[:, h, :], lhsT=kd[:, h, :], rhs=vn[:, h, :], start=True, stop=True)
            return ins
        S.op("pe", mm3, reads=[bkd, bvn], writes=bp3)
        S.op("dve", lambda e: e.tensor_tensor(out=S32[:], in0=S32[:], in1=lb[0:64, tb, :].unsqueeze(2).to_broadcast([64, 6, 64]), op=ALU.mult),
             reads=[bS32, blb], writes=[bS32])
        S.op("dve", lambda e: e.tensor_tensor(out=S32[:], in0=S32[:], in1=p36, op=ALU.add), reads=[bS32] + bp3, writes=[bS32])
        S.op("act", lambda e: e.activation(out=Sb[:], in_=S32[:], func=AF.Copy), reads=[bS32], writes=[bSb])
        yield
        S.op("dve", lambda e: e.tensor_tensor(out=o[:], in0=p2a6, in1=bc64(eG[:, tb, :]), op=ALU.mult), reads=bp2a + [beG], writes=[bo])
        S.op("dve", lambda e: e.tensor_tensor(out=o[:], in0=o[:], in1=p2b6, op=ALU.add), reads=[bo] + bp2b, writes=[bo])
        for h in range(6):
            S.op("act", lambda e, h=h: e.activation(out=osq[:, h, :], in_=o[:, h, :], func=AF.Square, accum_out=oss[:, h:h + 1]), reads=[bo], writes=[bosq, boss])
        S.op("act", lambda e: e.activation(out=ors[:], in_=oss[:], func=AF.Sqrt, scale=1.0 / 64, bias=EPS), reads=[boss], writes=[bors])
        S.op("dve", lambda e: e.reciprocal(out=ors[:], in_=ors[:]), reads=[bors], writes=[bors])
        S.op("dve", lambda e: e.tensor_tensor(out=o[:], in0=o[:], in1=bc64(ors[:]), op=ALU.mult), reads=[bo, bors], writes=[bo])
        S.op("dve", lambda e: e.tensor_tensor(out=o[:], in0=o[:], in1=onorm.unsqueeze(1).to_broadcast([128, 6, 64]), op=ALU.mult), reads=[bo, bsp], writes=[bo])
        S.op("act", lambda e: e.activation(out=zs[:], in_=ztok[:, tb, :], func=AF.Silu), reads=[bztok], writes=[bzs])
        S.op("dve", lambda e: e.tensor_tensor(out=of[:], in0=o[:].rearrange("p h d -> p (h d)"), in1=zs[:], op=ALU.mult), reads=[bo, bzs], writes=[bof])
        yield
        pO, bpO = self.ps_half("dn")
        pOb = pO.bitcast(BF16)

        def trO(e):
            ins = None
            for c in range(3):
                ins = e.transpose(out=pOb[:, c * 128:(c + 1) * 128], in_=of[:, c * 128:(c + 1) * 128], identity=ident_b)
            return ins
        S.op("pe", trO, reads=[bof] + CB, writes=bpO)
        S.op("act", lambda e: e.activation(out=mixT[:, 0:3, ts], in_=pOb[:, 0:384].rearrange("p (c t) -> p c t", c=3), func=AF.Copy),
             reads=bpO, writes=bmix[0:3])
        yield

    def _fox_head(self, I, h, fxq, bfxq, KT, bKT, V1, bV1, bias_tab, bbias, Es, rr, brr, mixT, bmix):
        S = self.S
        nj = NTB * I + NTB
        ones_f = self.ones_f
        c = h // 2
        hp = slice((h % 2) * 64, (h % 2) * 64 + 64)
        po_, bpo = self.ps_resv()
        po = po_[:, 0:SBM]

        def score(j):
            ps__, bps_ = self.ps_half("fx")
            ps_ = ps__[:, 0:SBM]
            S.op("pe", lambda e: e.matmul(out=ps_, lhsT=KT[hp, c, j * 128:(j + 1) * 128], rhs=fxq[hp, c, :], start=True, stop=True),
                 reads=[bKT, bfxq[c]], writes=bps_)
            return ps_, bps_

        def pv(j, E, bE):
            S.op("pe", lambda e: e.matmul(out=po[0:65, :], lhsT=V1[:, j, h, :], rhs=E[:], start=(j == 0), stop=(j == nj - 1)),
                 reads=[bE, bV1], writes=bpo)

        def expo(j, E, bE, ps_, bps_):
            S.op("act", lambda e: e.activation(out=E[:], in_=ps_, func=AF.Exp, bias=bias_tab[:, j, h:h + 1]), reads=bps_ + [bbias], writes=[bE])
            if j >= NTB * I:
                dm = self.dmask_b[j - NTB * I]
                S.op("pool", lambda e: e.tensor_tensor(out=E[:], in0=E[:], in1=dm, op=ALU.mult), reads=[bE] + self.CB, writes=[bE])

        nxt = score(0)
        for j in range(nj):
            ps_, bps_ = nxt
            E, bE = Es[(h * 64 + j) % 3]
            expo(j, E, bE, ps_, bps_)
            if j + 1 < nj:
                nxt = score(j + 1)
            pv(j, E, bE)
            yield
        S.op("dve", lambda e: e.reciprocal(out=rr[64:65, :], in_=po[64:65, :]), reads=bpo + [brr], writes=[brr])
        pr_, bpr = self.ps_half("fx")
        pr = pr_[:, 0:SBM]
        S.op("pe", lambda e: e.matmul(out=pr[0:64, :], lhsT=ones_f[64:65, 0:64], rhs=rr[64:65, :], start=True, stop=True),
             reads=[brr] + self.CB, writes=bpr)
        S.op("act", lambda e: e.activation(out=rr[0:64, :], in_=pr[0:64, :], func=AF.Copy), reads=bpr + [brr], writes=[brr])
        S.op("dve", lambda e: e.tensor_tensor(out=mixT[hp, 3 + c, :], in0=po[0:64, :], in1=rr[0:64, :], op=ALU.mult), reads=bpo + [brr], writes=[bmix[3 + c]])
        yield

    def _phase_ffn(self, l, x1, bx1, xout, bxout):
        nc, S = self.nc, self.S
        CB = self.CB
        cast_todo = self._cast_for_next_layer(self.layers.index(l))
        n_per = -(-len(cast_todo) // 7) if cast_todo else 0
        moe = (l % 2 == 1)
        with contextlib.ExitStack() as ph:
            sb = lambda name, shape, dt: self.sb(ph, name, shape, dt)
            spt = sb("fspt", [128, SP_COLS], F32); bsp = Buf()
            S.dma("sp", spt[:], self.d_sp[l * 128:(l + 1) * 128, :], writes=[bsp])
            g2T = spt[:, 8:16]
            acc = sb("acc", [128, 4, D], F32); bacc = [Buf() for _ in range(4)]
            nwork = (None, None, sb("fn_ss", [128, 1], F32), Buf(), sb("fn_rs", [128, 1], F32), Buf(),
                     sb("fn_xn", [128, D], BF16), Buf())
            h2T = sb("h2T", [128, 8, SB], BF16); bh2T = Buf()
            wgus = [(sb(f"wgu{i}", [128, 2, 8, 256], BF16), Buf()) for i in range(2)]
            nfd = 12 if moe else 11
            wds = [(sb(f"wd{i}", [128, nfd, D], BF16), Buf()) for i in range(2)]
            nact = 12 if moe else 22
            actT = sb("actT", [128, nact, SB], BF16); bact = [Buf() for _ in range(nact)]
            sgs = [(sb(f"sg{i}", [128, SB], F32), Buf()) for i in range(2)]
            if moe:
                rt = sb("rt", [128, 8, 8], F32); brt = Buf()
                S.dma("sp", rt[:].rearrange("p a b -> p (a b)"), self.d_router, writes=[brt])
                h2Tf = sb("h2Tf", [128, 8, 128], F32); bh2Tf = Buf()
                xf = sb("xf", [128, D], F32); bxf = Buf()
                lg = sb("lg", [128, 4, 8], F32); blg = Buf()
                gates = sb("gates", [128, 4, 8], F32); bgates = Buf()
                gw = {k: (sb("gw_" + k, [128, 4, 8], F32), Buf()) for k in ("m1", "l2", "m2")}
                gs = {k: (sb("gs_" + k, [128, 4], F32), Buf()) for k in ("m1", "m2", "s", "s2")}
            wdi = 0
            wgi = 0
            for I in range(NSB):
                t0 = I * SB
                for tb in range(4):
                    S.dma("sp", acc[:, tb, :], x1[t0 + tb * 128:t0 + (tb + 1) * 128, :], reads=[bx1], writes=[bacc[tb]])
                    want = (h2Tf[:], bh2Tf, xf, bxf) if moe else None
                    self._norm_T(acc[:, tb, :], bacc[tb], g2T, bsp, h2T[:, :, tb * 128:(tb + 1) * 128], bh2T, nwork, want_f32=want)
                    if moe:
                        pl, bpl = self.ps_half()

                        def mml(e, pl=pl):
                            ins = None
                            for kc in range(8):
                                ins = e.matmul(out=pl[:, 0:8], lhsT=h2Tf[:, kc, :], rhs=rt[:, kc, :], start=(kc == 0), stop=(kc == 7))
                            return ins
                        S.op("pe", mml, reads=[bh2Tf, brt], writes=bpl)
                        S.op("dve", lambda e, pl=pl, tb=tb: e.tensor_copy(out=lg[:, tb, :], in_=pl[:, 0:8]), reads=bpl, writes=[blg])
                if moe:
                    self._gates(lg, blg, gates, bgates, gw, gs)
                    if self.debug:
                        for tb in range(4):
                            S.dma("pool", self.dbg_g[t0 + tb * 128:t0 + (tb + 1) * 128, 0:8], lg[:, tb, :], reads=[blg])
                            S.dma("pool", self.dbg_g[t0 + tb * 128:t0 + (tb + 1) * 128, 8:16], gates[:, tb, :], reads=[bgates])
                nexp = 8 if moe else 1
                for ex in range(nexp):
                    ngt = 6 if moe else 11
                    for gt in range(ngt):
                        wgu, bwgu = wgus[wgi % 2]; wgi += 1
                        if moe:
                            row = (ex * 6 + gt) * 128
                            S.dma("sp", wgu[:].rearrange("p a b c -> p (a b c)"), self.b_mgu[row:row + 128, :], reads=[self.wbuf[("mgu", row)]], writes=[bwgu])
                        else:
                            row = gt * 128
                            S.dma("sp", wgu[:].rearrange("p a b c -> p (a b c)"), self.b_wgu[row:row + 128, :], reads=[self.wbuf[("wgu", row)]], writes=[bwgu])
                        for j in range(2):
                            fc = 2 * gt + j
                            pg, bpg = self.ps_half()
                            pu, bpu = self.ps_half()

                            def mmg(e, wgu=wgu, j=j, pg=pg, which=0):
                                ins = None
                                for kc in range(8):
                                    ins = e.matmul(out=pg, lhsT=wgu[:, which, kc, j * 128:(j + 1) * 128], rhs=h2T[:, kc, :], start=(kc == 0), stop=(kc == 7))
                                return ins
                            S.op("pe", mmg, reads=[bwgu, bh2T], writes=bpg)
                            S.op("pe", lambda e, wgu=wgu, j=j, pu=pu: mmg(e, wgu, j, pu, 1), reads=[bwgu, bh2T], writes=bpu)
                            sg, bsg = sgs[fc % 2]
                            S.op("act", lambda e, sg=sg, pg=pg: e.activation(out=sg[:], in_=pg, func=AF.Silu), reads=bpg, writes=[bsg])
                            S.op("dve", lambda e, sg=sg, pu=pu, fc=fc: e.tensor_tensor(out=actT[:, fc, :], in0=sg[:], in1=pu, op=ALU.mult),
                                 reads=[bsg] + bpu, writes=[bact[fc]])
                    ndt = 1 if moe else 2
                    for dti in range(ndt):
                        wd, bwd = wds[wdi % 2]; wdi += 1
                        if moe:
                            S.dma("sp", wd[:].rearrange("p a b -> p (a b)"), self.b_md[ex * 128:(ex + 1) * 128, :], reads=[self.wbuf[("md", ex * 128)]], writes=[bwd])
                        else:
                            S.dma("sp", wd[:].rearrange("p a b -> p (a b)"), self.b_wd[dti * 128:(dti + 1) * 128, :], reads=[self.wbuf[("wd", dti * 128)]], writes=[bwd])
                        for tb in range(4):
                            for nh in range(2):
                                pd, bpd = self.ps_half()

                                def mmd(e, wd=wd, tb=tb, nh=nh, pd=pd, dti=dti):
                                    ins = None
                                    for f in range(nfd):
                                        ins = e.matmul(out=pd, lhsT=actT[:, dti * nfd + f, tb * 128:(tb + 1) * 128], rhs=wd[:, f, nh * 512:(nh + 1) * 512],
                                                       start=(f == 0), stop=(f == nfd - 1))
                                    return ins
                                S.op("pe", mmd, reads=[bwd] + bact[dti * nfd:(dti + 1) * nfd], writes=bpd)
                                asl = acc[:, tb, nh * 512:(nh + 1) * 512]
                                if moe:
                                    S.op("dve", lambda e, asl=asl, pd=pd, tb=tb, ex=ex: e.scalar_tensor_tensor(out=asl, in0=pd, scalar=gates[:, tb, ex:ex + 1], in1=asl, op0=ALU.mult, op1=ALU.add),
                                         reads=bpd + [bgates, bacc[tb]], writes=[bacc[tb]])
                                else:
                                    S.op("dve", lambda e, asl=asl, pd=pd: e.tensor_tensor(out=asl, in0=asl, in1=pd, op=ALU.add), reads=bpd + [bacc[tb]], writes=[bacc[tb]])
                for tb in range(4):
                    S.dma("pool", xout[t0 + tb * 128:t0 + (tb + 1) * 128, :], acc[:, tb, :], reads=[bacc[tb]], writes=[bxout], add=True)
                for _ in range(n_per):
                    if cast_todo:
                        self._cast(*cast_todo.pop(0))
            while cast_todo:
                self._cast(*cast_todo.pop(0))
            S.barrier()
            with nc.named_scope(f"ffn{l}"):
                S.emit()

    def _gates(self, lg, blg, gates, bgates, gw, gs):
        S = self.S
        m1, bm1 = gs["m1"]; m2, bm2 = gs["m2"]; s, bs = gs["s"]; s2, bs2 = gs["s2"]
        k1, bk1 = gw["m1"]; l2, bl2 = gw["l2"]; k2, bk2 = gw["m2"]
        bc8 = lambda ap: ap.unsqueeze(2).to_broadcast([128, 4, 8])
        S.op("dve", lambda e: e.tensor_reduce(out=m1[:], in_=lg[:], axis=AX.X, op=ALU.max), reads=[blg], writes=[bm1])
        S.op("dve", lambda e: e.tensor_tensor(out=k1[:], in0=lg[:], in1=bc8(m1[:]), op=ALU.is_equal), reads=[blg, bm1], writes=[bk1])
        S.op("dve", lambda e: e.scalar_tensor_tensor(out=l2[:], in0=k1[:], scalar=-1e30, in1=lg[:], op0=ALU.mult, op1=ALU.add), reads=[bk1, blg], writes=[bl2])
        S.op("dve", lambda e: e.tensor_reduce(out=m2[:], in_=l2[:], axis=AX.X, op=ALU.max), reads=[bl2], writes=[bm2])
        S.op("dve", lambda e: e.tensor_tensor(out=k2[:], in0=l2[:], in1=bc8(m2[:]), op=ALU.is_equal), reads=[bl2, bm2], writes=[bk2])
        S.op("dve", lambda e: e.tensor_tensor(out=s[:], in0=m1[:], in1=m2[:], op=ALU.subtract), reads=[bm1, bm2], writes=[bs])
        S.op("act", lambda e: e.activation(out=s[:], in_=s[:], func=AF.Sigmoid), reads=[bs], writes=[bs])
        S.op("dve", lambda e: e.tensor_scalar(out=s2[:], in0=s[:], scalar1=-1.0, scalar2=1.0, op0=ALU.mult, op1=ALU.add), reads=[bs], writes=[bs2])
        S.op("dve", lambda e: e.tensor_tensor(out=k1[:], in0=k1[:], in1=bc8(s[:]), op=ALU.mult), reads=[bk1, bs], writes=[bk1])
        S.op("dve", lambda e: e.tensor_tensor(out=k2[:], in0=k2[:], in1=bc8(s2[:]), op=ALU.mult), reads=[bk2, bs2], writes=[bk2])
        S.op("dve", lambda e: e.tensor_tensor(out=gates[:], in0=k1[:], in1=k2[:], op=ALU.add), reads=[bk1, bk2], writes=[bgates])


_CACHE = {}


def _get_prog(debug=False, nlayers=2, stop_after=None, layers=None):
    key = (debug, nlayers, stop_after, layers)
    if key not in _CACHE:
        p = Prog(debug=debug, nlayers=nlayers, stop_after=stop_after, layers=layers)
        _CACHE[key] = p.build()
    return _CACHE[key]


def _run(inputs, debug=False, nlayers=2, stop_after=None, trace=False, layers=None):
    nc = _get_prog(debug, nlayers, stop_after, layers)
    lay = _layout_inputs(inputs)
    lay["cstf"], lay["cstb"] = _constants()
    x = np.ascontiguousarray(inputs["x"], dtype=np.float32)
    in_maps = []
    for b in range(8):
        m = dict(lay)
        m["x"] = x[b]
        in_maps.append(m)
    kw = {"trace": True} if trace else {}
    return run_bass_kernel_spmd(nc, in_maps, core_ids=list(range(8)), **kw)


def kernel(**inputs):
    res = _run(inputs)
    return np.stack([np.asarray(r["out"], dtype=np.float32) for r in res.results], axis=0)
```

```python
import contextlib
import os
import numpy as np
import concourse.bass as bass
import concourse.mybir as mybir
from concourse.bass_utils import run_bass_kernel_spmd

F32 = mybir.dt.float32
BF16 = mybir.dt.bfloat16
AF = mybir.ActivationFunctionType
ALU = mybir.AluOpType
AX = mybir.AxisListType

T = 4096
D = 1024
NSB = 8
SB = 512
SBM = 256
NSBM = T // SBM
NTB = SBM // 128
NEG = -30000.0
EPS = 1e-6
EPOCH = 30000
SAME_ENGINE_SYNC = True


class Buf:
    __slots__ = ("w", "r")

    def __init__(self):
        self.w = []
        self.r = []


class Sched:
    ENGS = ("pe", "dve", "act", "pool", "sp")

    def __init__(self, nc, stack, n_dma_sems=12):
        self.nc = nc
        self.stack = stack
        self.streams = {e: [] for e in self.ENGS}
        self.sem = {}
        self.cnt = {}
        self.waited = {e: {} for e in self.ENGS}
        self.nsem = 0
        for e in self.ENGS:
            self._new_epoch(e)
        self.dma_pool = {}
        self.n_dma_sems = n_dma_sems
        self.last = {e: None for e in self.ENGS}
        self.nops = 0

    def _alloc_sem(self, name):
        self.nsem += 1
        return self.stack.enter_context(self.nc.semaphore(name))

    def _new_epoch(self, e):
        self.sem[e] = self._alloc_sem(f"tl_{e}_{self.nsem}")
        self.cnt[e] = 0

    def _emit_waits(self, eng, deps):
        need = {}
        for tok in deps:
            if tok is None:
                continue
            sem, val, src = tok
            if src == eng and (eng == "pe" or not SAME_ENGINE_SYNC):
                continue
            k = id(sem)
            if k not in need or need[k][1] < val:
                need[k] = (sem, val)
        w = self.waited[eng]
        for k, (sem, val) in need.items():
            if w.get(k, 0) >= val:
                continue
            w[k] = val
            self.streams[eng].append(("wait", sem, val))

    @staticmethod
    def _deps(reads, writes):
        deps = []
        for b in reads:
            deps.extend(b.w)
        for b in writes:
            deps.extend(b.w)
            deps.extend(b.r)
        return deps

    @staticmethod
    def _mark(tok, reads, writes, add=False):
        for b in reads:
            b.r.append(tok)
            if len(b.r) > 24:
                b.r = b.r[-24:]
        for b in writes:
            if add:
                b.w.append(tok)
            else:
                b.w = [tok]
                b.r = []

    def op(self, eng, fn, reads=(), writes=()):
        self._emit_waits(eng, self._deps(reads, writes))
        if self.cnt[eng] >= EPOCH:
            self._new_epoch(eng)
        self.cnt[eng] += 1
        tok = (self.sem[eng], self.cnt[eng], eng)
        self.streams[eng].append(("op", fn, self.sem[eng], 1))
        self.last[eng] = tok
        self._mark(tok, reads, writes)
        self.nops += 1
        return tok

    def dma(self, q, out, in_, reads=(), writes=(), add=False):
        deps = self._deps(reads, () if add else writes)
        if add:
            for b in writes:
                deps.extend(b.r)
        if q not in self.dma_pool:
            self.dma_pool[q] = {"sems": [self._alloc_sem(f"dma_{q}_{i}") for i in range(self.n_dma_sems)],
                                "vals": [0] * self.n_dma_sems, "i": 0}
        p = self.dma_pool[q]
        i = p["i"]
        p["i"] = (i + 1) % self.n_dma_sems
        if p["vals"][i] + 16 > EPOCH:
            p["sems"][i] = self._alloc_sem(f"dma_{q}_{i}_{self.nsem}")
            p["vals"][i] = 0
        sem = p["sems"][i]
        if p["vals"][i] > 0:
            deps.append((sem, p["vals"][i], "dma"))
        self._emit_waits(q, deps)
        p["vals"][i] += 16
        tok = (sem, p["vals"][i], "dma")
        self.streams[q].append(("op", lambda e, out=out, in_=in_: e.dma_start(out=out, in_=in_), sem, 16))
        self._mark(tok, reads, writes, add=add)
        return tok

    def barrier(self):
        toks = [t for t in self.last.values() if t is not None]
        for q, p in self.dma_pool.items():
            for s, v in zip(p["sems"], p["vals"]):
                if v > 0:
                    toks.append((s, v, "dma"))
        for e in self.ENGS:
            self._emit_waits(e, toks)

    def emit(self):
        nc = self.nc
        streams = self.streams
        with nc.Block() as block:
            def run(e, name):
                for it in streams[name]:
                    if it[0] == "wait":
                        e.wait_ge(it[1], it[2])
                    else:
                        it[1](e).then_inc(it[2], it[3])

            @block.tensor
            def _(e):
                run(e, "pe")

            @block.vector
            def _(e):
                run(e, "dve")

            @block.scalar
            def _(e):
                run(e, "act")

            @block.gpsimd
            def _(e):
                run(e, "pool")

            @block.sync
            def _(e):
                run(e, "sp")
        self.streams = {e: [] for e in self.ENGS}


N_IN = 2962
SP_COLS = 256


def _layout_inputs(inp):
    out = {}
    f = lambda a: np.ascontiguousarray(a, dtype=np.float32)
    o_q, o_k, o_v, o_z, o_a, o_b = 0, 384, 768, 1152, 1536, 1542
    o_fq, o_fk, o_fv, o_ff, o_pl = 1548, 1932, 2316, 2700, 2706
    cols = np.concatenate([
        np.arange(o_q, o_q + 384), np.arange(o_k, o_k + 384), np.arange(o_v, o_v + 384),
        np.arange(o_fq, o_fq + 384), np.arange(o_fk, o_fk + 384), np.arange(o_pl, o_pl + 256),
        np.arange(o_fv, o_fv + 384), np.arange(o_z, o_z + 384),
        np.arange(o_a, o_a + 6), np.arange(o_b, o_b + 6), np.arange(o_ff, o_ff + 6)])
    wi = np.zeros((2, 1024, 3072), np.float32)
    wi[:, :, :cols.size] = inp["w_in"][:, :, cols]
    wi = wi.reshape(2, 8, 128, 6, 512).transpose(0, 3, 2, 1, 4)
    out["wi"] = f(wi.reshape(2 * 6 * 128, 8 * 512))
    wo = inp["w_out"].reshape(2, 8, 128, 1024).transpose(0, 2, 1, 3)
    out["wo"] = f(wo.reshape(2 * 128, 8 * 1024))
    g = inp["ffn_gate"][0].reshape(8, 128, 11, 256)
    u = inp["ffn_up"][0].reshape(8, 128, 11, 256)
    gu = np.stack([g, u], 0).transpose(3, 2, 0, 1, 4)
    out["wgu"] = f(gu.reshape(11 * 128, 2 * 8 * 256))
    dn = inp["ffn_down"][0].reshape(2, 11, 128, 1024).transpose(0, 2, 1, 3)
    out["wd"] = f(dn.reshape(2 * 128, 11 * 1024))
    g = inp["moe_gate"][0].reshape(8, 8, 128, 6, 256)
    u = inp["moe_up"][0].reshape(8, 8, 128, 6, 256)
    gu = np.stack([g, u], 0).transpose(1, 4, 3, 0, 2, 5)
    out["mgu"] = f(gu.reshape(8 * 6 * 128, 2 * 8 * 256))
    dn = inp["moe_down"][0].reshape(8, 12, 128, 1024).transpose(0, 2, 1, 3)
    out["md"] = f(dn.reshape(8 * 128, 12 * 1024))
    out["router"] = f(inp["router"][0].reshape(8, 128, 8).transpose(1, 0, 2).reshape(128, 64))
    sp = np.zeros((2, 128, SP_COLS), np.float32)
    bc = lambda v: np.broadcast_to(np.asarray(v)[None, :], (128, len(v)))
    for l in range(2):
        c = 0
        sp[l, :, c:c + 8] = inp["norm1"][l].reshape(8, 128).T; c += 8
        sp[l, :, c:c + 8] = inp["norm2"][l].reshape(8, 128).T; c += 8
        sp[l, :, c:c + 36] = inp["dn_conv"][l].reshape(4, 9, 128).transpose(2, 1, 0).reshape(128, 36); c += 36
        sp[l, :, c:c + 6] = bc(inp["dn_a_log"][l]); c += 6
        sp[l, :, c:c + 6] = bc(inp["dn_dt_bias"][l]); c += 6
        sp[l, :, c:c + 64] = bc(inp["dn_onorm"][l]); c += 64
        sp[l, :, c] = np.tile(inp["fx_qnorm"][l], 2); c += 1
        sp[l, :, c] = np.tile(inp["fx_knorm"][l], 2); c += 1
        sp[l, :, c:c + 6] = bc(inp["fx_f_bias"][l]); c += 6
        sp[l, :, c:c + 2] = inp["pool_scale"][l].reshape(2, 128).T; c += 2
    out["sp"] = f(sp.reshape(2 * 128, SP_COLS))
    pw = np.zeros((2, 128, 2, 128), np.float32)
    for l in range(2):
        for c in range(2):
            pw[l, 0:64, c, 0:64] = inp["pool_w"][l, 2 * c]
            pw[l, 64:128, c, 64:128] = inp["pool_w"][l, 2 * c + 1]
    out["pw"] = f(pw.reshape(2 * 128, 256))
    return out


CSTF_COLS = 128 * 4 + 4 + 32
CSTB_COLS = 128 * 9 + 2 * SBM + 128


def _constants():
    cf = np.zeros((128, CSTF_COLS), np.float32)
    cb = np.zeros((128, CSTB_COLS), np.float32)
    j = np.arange(128)[:, None]
    i = np.arange(128)[None, :]
    cf[:, 0:128] = np.eye(128)
    cf[:, 128:256] = (j <= i)
    cf[:, 256:384] = 1.0
    cf[:, 384:512] = np.where(i > j, 0.0, NEG)
    cf[:, 512] = np.where(np.arange(128) < 64, 1 / 2, 1 / 4)
    cf[:, 513] = np.where(np.arange(128) < 64, 1 / 8, 1 / 16)
    wins = np.array([[2, 4], [8, 16]])
    for ch in range(2):
        w = np.where(np.arange(128) < 64, wins[ch, 0], wins[ch, 1])[:, None]
        cf[:, 516 + 16 * ch:516 + 16 * (ch + 1)] = 1.0 / np.minimum(np.arange(16)[None, :] + 1, w)
    cb[:, 0:128] = np.eye(128)
    cb[:, 128:256] = (j // 64 == i // 64)
    o = 256
    for l in range(7):
        b = 1 << l
        m = (j // (2 * b) == i // (2 * b)) & ((j % (2 * b)) < b) & ((i % (2 * b)) >= b)
        cb[:, o:o + 128] = -1.0 * m; o += 128
    t = np.arange(SBM)[None, :]
    for m in range(NTB):
        cb[:, o:o + SBM] = (j + 128 * m <= t); o += SBM
    m0 = (j // 2 == i // 2) & ((j % 2) < 1) & ((i % 2) >= 1)
    cb[:, o:o + 128] = -1.0 * m0.T
    return cf, cb


class Prog:
    def __init__(self, debug=False, nlayers=2, stop_after=None, layers=None):
        self.debug = debug
        self.nlayers = nlayers
        self.stop_after = stop_after
        self.layers = tuple(layers) if layers is not None else tuple(range(nlayers))

    def build(self):
        nc = bass.Bass("TRN2", target_bir_lowering=False)
        self.nc = nc
        dt_in = lambda name, shape: nc.dram_tensor(name, list(shape), F32, kind="ExternalInput").ap()
        self.x_in = dt_in("x", (T, D))
        self.d_wi = dt_in("wi", (2 * 6 * 128, 4096))
        self.d_wo = dt_in("wo", (256, 8192))
        self.d_wgu = dt_in("wgu", (11 * 128, 4096))
        self.d_wd = dt_in("wd", (256, 11264))
        self.d_mgu = dt_in("mgu", (48 * 128, 4096))
        self.d_md = dt_in("md", (8 * 128, 12288))
        self.d_router = dt_in("router", (128, 64))
        self.d_sp = dt_in("sp", (256, SP_COLS))
        self.d_pw = dt_in("pw", (256, 256))
        self.d_cstf = dt_in("cstf", (128, CSTF_COLS))
        self.d_cstb = dt_in("cstb", (128, CSTB_COLS))
        self.out = nc.dram_tensor("out", [T, D], F32, kind="ExternalOutput").ap()
        kind_dbg = "ExternalOutput" if self.debug else "Internal"
        self.x1 = [nc.dram_tensor(f"x1_{l}", [T, D], F32, kind=kind_dbg).ap() for l in range(2)]
        self.x2 = nc.dram_tensor("x2_0", [T, D], F32, kind=kind_dbg).ap()
        self.dbg_g = nc.dram_tensor("dbg_g", [T, 16], F32, kind=kind_dbg).ap()
        scr = lambda name, ap: nc.dram_tensor(name, list(ap.shape), BF16, kind="Internal").ap()
        self.b_wi = scr("b_wi", self.d_wi)
        self.b_wo = scr("b_wo", self.d_wo)
        self.b_wgu = scr("b_wgu", self.d_wgu)
        self.b_wd = scr("b_wd", self.d_wd)
        self.b_mgu = scr("b_mgu", self.d_mgu)
        self.b_md = scr("b_md", self.d_md)
        self.wbuf = {}

        with contextlib.ExitStack() as st:
            self.st = st
            S = self.S = Sched(nc, st)
            self._alloc_persistent(st)
            self._load_consts()
            l_first = self.layers[0]
            self._cast("wi", l_first * 768, (l_first + 1) * 768)
            self._cast("wo", l_first * 128, (l_first + 1) * 128)
            xin = self.x_in
            bx_in = Buf()
            for li, l in enumerate(self.layers):
                bx1 = Buf()
                self._phase_mixer(l, xin, bx_in, self.x1[l], bx1)
                if self.stop_after == ("M", l):
                    break
                xout = self.x2 if (li + 1 < len(self.layers)) else self.out
                bxo = Buf()
                self._phase_ffn(l, self.x1[l], bx1, xout, bxo)
                xin, bx_in = xout, bxo
            S.barrier()
            S.emit()
        return nc

    def _alloc_persistent(self, st):
        nc = self.nc
        self.psum = []
        self.psb = []
        for i in range(4):
            t = st.enter_context(nc.psum_tensor(f"ps{i}", [128, 1024], F32))
            self.psum.append(t)
            self.psb.append((Buf(), Buf()))
        self.ps_h = 0
        self.ps_f = 0
        self.cst = st.enter_context(nc.sbuf_tensor("s_cstf", [128, CSTF_COLS], F32))
        self.cstb = st.enter_context(nc.sbuf_tensor("s_cstb", [128, CSTB_COLS], BF16))
        self.b_cst = Buf()
        self.b_cstb = Buf()

    def ps_resv(self):
        return self.psum[3][:, 512:1024], [self.psb[3][1]]

    def _half(self, i):
        t = self.psum[i // 2]
        lo = (i % 2) * 512
        return t[:, lo:lo + 512], [self.psb[i // 2][i % 2]]

    def ps_half(self, pool=None):
        if pool == "fx":
            self._fxh = (getattr(self, "_fxh", 0) + 1) % 3
            return self._half(4 + self._fxh)
        if pool == "dn":
            self._dnh = (getattr(self, "_dnh", 0) + 1) % 4
            return self._half(self._dnh)
        i = self.ps_h
        self.ps_h = (i + 1) % 7
        return self._half(i)

    def ps_full(self, pool=None):
        if pool == "dn":
            self._dnf = (getattr(self, "_dnf", 0) + 1) % 2
            i = self._dnf
            return self.psum[i][:, :], list(self.psb[i])
        i = self.ps_f
        self.ps_f = (i + 1) % 3
        return self.psum[i][:, :], list(self.psb[i])

    def sb(self, st, name, shape, dt):
        self._uid = getattr(self, "_uid", 0) + 1
        return st.enter_context(self.nc.sbuf_tensor(f"s{self._uid}_{name}", list(shape), dt))

    def _cast(self, name, r0=None, r1=None):
        S = self.S
        src, dst = {"wi": (self.d_wi, self.b_wi), "wo": (self.d_wo, self.b_wo), "wgu": (self.d_wgu, self.b_wgu),
                    "wd": (self.d_wd, self.b_wd), "mgu": (self.d_mgu, self.b_mgu), "md": (self.d_md, self.b_md)}[name]
        rows = src.shape[0]
        r0 = 0 if r0 is None else r0
        r1 = rows if r1 is None else r1
        for r in range(r0, r1, 128):
            n = min(128, r1 - r)
            bb = Buf()
            self.wbuf[(name, r)] = bb
            S.dma("pool", dst[r:r + n, :], src[r:r + n, :], writes=[bb])

    def _cast_for_ffn(self, l):
        if l % 2 == 0:
            self._cast("wgu")
            self._cast("wd")
        else:
            if ("mgu", 0) not in self.wbuf:
                self._cast("mgu")
                self._cast("md")

    def _cast_for_next_layer(self, li):
        todo = []
        for l2 in self.layers[li + 1:]:
            if ("wi", l2 * 768) not in self.wbuf:
                todo += [("wi", r, r + 128) for r in range(l2 * 768, (l2 + 1) * 768, 128)]
                todo += [("wo", l2 * 128, (l2 + 1) * 128)]
            if l2 % 2 == 1 and ("mgu", 0) not in self.wbuf:
                todo += [("mgu", r, r + 128) for r in range(0, 48 * 128, 128)]
                todo += [("md", r, r + 128) for r in range(0, 8 * 128, 128)]
        return todo

    def _load_consts(self):
        S = self.S
        S.dma("sp", self.cst[:], self.d_cstf, writes=[self.b_cst])
        S.dma("pool", self.cstb[:], self.d_cstb, writes=[self.b_cstb])
        cst, cstb = self.cst, self.cstb
        self.ident_f = cst[:, 0:128]
        self.tri_f = cst[:, 128:256]
        self.ones_f = cst[:, 256:384]
        self.negstrict = cst[:, 384:512]
        self.pinvw = cst[:, 512:514]
        self.pinvc = [cst[:, 516 + 16 * c:516 + 16 * (c + 1)] for c in range(2)]
        self.ident_b = cstb[:, 0:128]
        self.blk2_b = cstb[:, 128:256]
        self.negM_b = [cstb[:, 256 + 128 * l:256 + 128 * (l + 1)] for l in range(7)]
        self.dmask_b = [cstb[:, 1152 + SBM * m:1152 + SBM * (m + 1)] for m in range(NTB)]
        self.negM0T_b = cstb[:, 1152 + SBM * NTB:1152 + SBM * NTB + 128]
        self.CB = [self.b_cst, self.b_cstb]

    def _norm_T(self, xt, bxt, gT, bg, hT_dst, bhT, work, want_f32=None):
        S = self.S
        sq, bsq, ss, bss, rs, brs, xn, bxn = work
        S.op("act", lambda e: e.activation(out=xn[:], in_=xt, func=AF.Square, accum_out=ss[:]), reads=[bxt], writes=[bxn, bss])
        S.op("act", lambda e: e.activation(out=rs[:], in_=ss[:], func=AF.Sqrt, scale=1.0 / D, bias=EPS), reads=[bss], writes=[brs])
        S.op("dve", lambda e: e.reciprocal(out=rs[:], in_=rs[:]), reads=[brs], writes=[brs])
        S.op("act", lambda e: e.activation(out=xn[:], in_=xt, func=AF.Copy, scale=rs[:, 0:1]), reads=[bxt, brs], writes=[bxn])
        pT, bpT = self.ps_half()
        pTb = pT.bitcast(BF16).rearrange("p (a b) -> p a b", a=8)
        ident_b = self.ident_b

        def tr(e):
            ins = None
            for c in range(8):
                ins = e.transpose(out=pTb[:, c, :], in_=xn[:, c * 128:(c + 1) * 128], identity=ident_b)
            return ins
        S.op("pe", tr, reads=[bxn] + self.CB, writes=bpT)
        S.op("dve", lambda e: e.tensor_tensor(out=hT_dst, in0=pTb, in1=gT.unsqueeze(2).to_broadcast([128, 8, 128]), op=ALU.mult),
             reads=bpT + [bg], writes=[bhT])
        if want_f32 is not None:
            dstf, bdf, xf, bxf = want_f32
            S.op("dve", lambda e: e.tensor_scalar(out=xf[:], in0=xt, scalar1=rs[:, 0:1], scalar2=None, op0=ALU.mult),
                 reads=[bxt, brs], writes=[bxf])
            pF, bpF = self.ps_full()
            pFv = pF.rearrange("p (a b) -> p a b", a=8)
            ident_f = self.ident_f

            def trf(e):
                ins = None
                for c in range(8):
                    ins = e.transpose(out=pFv[:, c, :], in_=xf[:, c * 128:(c + 1) * 128], identity=ident_f)
                return ins
            S.op("pe", trf, reads=[bxf] + self.CB, writes=bpF)
            S.op("dve", lambda e: e.tensor_tensor(out=dstf, in0=pFv, in1=gT.unsqueeze(2).to_broadcast([128, 8, 128]), op=ALU.mult),
                 reads=bpF + [bg], writes=[bdf])

    def _phase_mixer(self, l, xin, bxin, x1, bx1):
        nc, S = self.nc, self.S
        CB = self.CB
        with contextlib.ExitStack() as ph:
            sb = lambda name, shape, dt: self.sb(ph, name, shape, dt)
            spt = sb("spt", [128, SP_COLS], F32); bsp = Buf()
            S.dma("sp", spt[:], self.d_sp[l * 128:(l + 1) * 128, :], writes=[bsp])
            g1T = spt[:, 0:8]
            convw = spt[:, 16:52].rearrange("p (c j) -> p c j", c=9)
            alog = spt[:, 52:58]; dtb = spt[:, 58:64]
            onorm = spt[:, 64:128]
            fxqg = spt[:, 128:129]; fxkg = spt[:, 129:130]
            fbias = spt[:, 130:136]
            pscale = spt[:, 136:138]
            drv = sb("drv", [128, 16], F32); bdrv = Buf()
            S.op("act", lambda e: e.activation(out=drv[:, 0:6], in_=alog, func=AF.Exp), reads=[bsp], writes=[bdrv])
            S.op("dve", lambda e: e.tensor_scalar(out=drv[:, 0:6], in0=drv[:, 0:6], scalar1=-1.0, scalar2=None, op0=ALU.mult), reads=[bdrv], writes=[bdrv])
            S.op("dve", lambda e: e.tensor_scalar(out=drv[:, 8:9], in0=fxqg, scalar1=0.125, scalar2=None, op0=ALU.mult), reads=[bsp, bdrv], writes=[bdrv])
            aneg = drv[:, 0:6]; fxqg8 = drv[:, 8:9]
            pwf = sb("pwf", [128, 256], F32); bpwf = Buf()
            pwb = sb("pwb", [128, 2, 128], BF16); bpwb = Buf()
            S.dma("sp", pwf[:], self.d_pw[l * 128:(l + 1) * 128, :], writes=[bpwf])
            S.op("dve", lambda e: e.tensor_copy(out=pwb[:].rearrange("p a b -> p (a b)"), in_=pwf[:]), reads=[bpwf], writes=[bpwb])
            wo = sb("wo", [128, 8, 1024], BF16); bwo = Buf()
            S.dma("sp", wo[:].rearrange("p a b -> p (a b)"), self.b_wo[l * 128:(l + 1) * 128, :], reads=[self.wbuf[("wo", l * 128)]], writes=[bwo])
            KT = sb("KT", [128, 3, T], BF16); bKT = Buf()
            V1 = sb("V1", [128, 32, 6, 65], BF16); bV1 = Buf()
            S.op("pool", lambda e: e.memset(V1[:].rearrange("p a b c -> p (a b c)"), 1.0), writes=[bV1])
            c_all = sb("c_all", [128, 32, 6], F32); bcall = Buf()
            carry = sb("carry", [128, 6], F32); bcarry = Buf()
            S.op("pool", lambda e: e.memset(carry[:], 0.0), writes=[bcarry])
            dhalo = sb("dhalo", [128, 9, 3], F32); bdhalo = Buf()
            S.op("pool", lambda e: e.memset(dhalo[:].rearrange("p a b -> p (a b)"), 0.0), writes=[bdhalo])
            phalo = sb("phalo", [128, 2, 16], F32); bphalo = Buf()
            S.op("pool", lambda e: e.memset(phalo[:].rearrange("p a b -> p (a b)"), 0.0), writes=[bphalo])
            S32 = sb("S32", [64, 6, 64], F32); bS32 = Buf()
            Sb = sb("Sb", [64, 6, 64], BF16); bSb = Buf()
            S.op("pool", lambda e: e.memset(S32[:].rearrange("p a b -> p (a b)"), 0.0), writes=[bS32])
            S.op("pool", lambda e: e.memset(Sb[:].rearrange("p a b -> p (a b)"), 0.0), writes=[bSb])
            id6 = sb("id6", [128, 6, 128], BF16); bid6 = Buf()
            S.op("dve", lambda e: e.tensor_copy(out=id6[:], in_=self.ident_b.unsqueeze(1).to_broadcast([128, 6, 128])), reads=CB, writes=[bid6])
            xts = [(sb(f"xt{i}", [128, D], F32), Buf()) for i in range(2)]
            nwork = (None, None, sb("n_ss", [128, 1], F32), Buf(), sb("n_rs", [128, 1], F32), Buf(),
                     sb("n_xn", [128, D], BF16), Buf())
            hT = sb("hT", [128, 8, SBM], BF16); bhT = Buf()
            wis = [(sb(f"wi{i}", [128, 8, 512], BF16), Buf()) for i in range(2)]
            cin = [(sb(f"cin{i}", [128, 3 + SBM], F32), Buf()) for i in range(2)]
            cacc = [(sb(f"cacc{i}", [128, SBM], F32), Buf()) for i in range(2)]
            sqb = [(sb(f"sqb{i}", [128, SBM], BF16), Buf()) for i in range(2)]
            rsd = [(sb(f"rsd{i}", [128, SBM], F32), Buf()) for i in range(2)]
            dnT = sb("dnT", [128, 9, SBM], BF16); bdnT = [Buf() for _ in range(9)]
            qal = sb("qal", [64, 6, SBM], BF16); bqal = Buf()
            kal = sb("kal", [64, 6, SBM], BF16); bkal = Buf()
            fxq = sb("fxq", [128, 3, SBM], BF16); bfxq = [Buf() for _ in range(3)]
            ktok = sb("ktok", [128, NTB, 384], BF16); bktok = Buf()
            vtok = sb("vtok", [128, NTB, 384], BF16); bvtok = Buf()
            ztok = sb("ztok", [128, NTB, 384], F32); bztok = Buf()
            smt = sb("smt", [128, NTB, 18], F32); bsmt = Buf()
            pin = [(sb(f"pin{i}", [128, 16 + SBM], F32), Buf()) for i in range(2)]
            mixT = sb("mixT", [128, 8, SBM], BF16); bmix = [Buf() for _ in range(8)]
            x1t = [(sb(f"x1t{i}", [128, D], F32), Buf()) for i in range(2)]
            sm = {k: (sb("sm_" + k, [128, NTB, 6], F32), Buf()) for k in
                  ("g", "beta", "logf", "t", "G", "Gl", "eG", "ed", "lb", "cum", "tot", "cref")}
            bias_tab = sb("bias_tab", [128, 32, 6], F32); bbias = Buf()
            W = {}
            for k, shape, dt in (("diff", [128, 6, 128], F32), ("es", [128, 6, 128], BF16), ("ec", [128, 6, 128], BF16),
                                 ("Bp", [128, 6, 128], BF16), ("Ap", [128, 6, 128], BF16),
                                 ("qkT", [128, 6, 128], BF16), ("Xn", [128, 6, 128], BF16),
                                 ("Ua", [128, 6, 128], BF16), ("Ub", [128, 6, 128], BF16),
                                 ("Ta", [128, 6, 128], BF16), ("Tb", [128, 6, 128], BF16),
                                 ("kg", [128, 6, 64], BF16), ("kd", [128, 6, 64], BF16),
                                 ("ut", [128, 6, 64], F32), ("wT", [64, 6, 128], BF16),
                                 ("vn", [128, 6, 64], BF16),
                                 ("o", [128, 6, 64], F32), ("osq", [128, 6, 64], F32), ("oss", [128, 6], F32),
                                 ("ors", [128, 6], F32), ("zs", [128, 384], F32), ("of", [128, 384], BF16)):
                W[k] = (sb("w_" + k, shape, dt), Buf())
            Es = [(sb(f"E{i}", [128, SBM], BF16), Buf()) for i in range(3)]
            rr = sb("rr", [128, SBM], F32); brr = Buf()
            ps2 = sb("ps2", [128, 16 + SBM], F32); bps2 = Buf()
            ps4 = sb("ps4", [128, 16 + SBM], F32); bps4 = Buf()
            ps8 = sb("ps8", [128, 16 + SBM], F32); bps8 = Buf()
            pW = sb("pW", [128, SBM], F32); bpW = Buf()
            py = sb("py", [128, SBM], BF16); bpy = Buf()
            ident_b = self.ident_b

            _st = set(os.environ.get('MK_STAGES', 'C,D,E,F,G').split(','))
            for I in range(min(NSBM, int(os.environ.get('MK_NSB', NSBM)))):
                t0 = I * SBM
                for tb in range(NTB):
                    xt, bxt = xts[tb % 2]
                    S.dma("sp", xt[:], xin[t0 + tb * 128:t0 + (tb + 1) * 128, :], reads=[bxin], writes=[bxt])
                    self._norm_T(xt[:], bxt, g1T, bsp, hT[:, :, tb * 128:(tb + 1) * 128], bhT, nwork)
                pend = None
                for t in range(6):
                    wi, bwi = wis[t % 2]
                    row = (l * 6 + t) * 128
                    S.dma("sp", wi[:].rearrange("p a b -> p (a b)"), self.b_wi[row:row + 128, :], reads=[self.wbuf[("wi", row)]], writes=[bwi])
                    nfm = 4 if t < 4 else (1 if t == 4 else 0)
                    for j in range(nfm):
                        fc = 4 * t + j
                        pp_, bpp = self.ps_half()
                        pp = pp_[:, 0:SBM]

                        def mm(e, wi=wi, j=j, pp=pp):
                            ins = None
                            for kc in range(8):
                                ins = e.matmul(out=pp, lhsT=wi[:, kc, j * 128:(j + 1) * 128], rhs=hT[:, kc, :], start=(kc == 0), stop=(kc == 7))
                            return ins
                        S.op("pe", mm, reads=[bwi, bhT], writes=bpp)
                        if fc < 9:
                            gen = self._dn_conv(fc, pp, bpp, cin[fc % 2], cacc[fc % 2], sqb[fc % 2], rsd[fc % 2], dhalo, bdhalo,
                                                convw, bsp, dnT, bdnT)
                        elif fc < 15:
                            gen = self._fx_qk(fc - 9, I, pp, bpp, sqb[fc % 2], rsd[fc % 2], fxqg8, bdrv, fxkg, bsp, fxq, bfxq, KT, bKT)
                        else:
                            gen = self._pool_mix(fc - 15, I, pp, bpp, pin[fc % 2], phalo, bphalo, ps2, bps2, ps4, bps4, ps8, bps8,
                                                 pW, bpW, py, bpy, pwb, bpwb, pscale, bsp, mixT, bmix)
                        alive = next(gen, "done") != "done"
                        if pend is not None:
                            for _ in pend:
                                pass
                        pend = gen if alive else None
                    if t == 4:
                        for tb in range(NTB):
                            pp, bpp = self.ps_half()

                            def mm(e, wi=wi, tb=tb, pp=pp):
                                ins = None
                                for kc in range(8):
                                    ins = e.matmul(out=pp[:, 0:384], lhsT=hT[:, kc, tb * 128:(tb + 1) * 128], rhs=wi[:, kc, 128:512], start=(kc == 0), stop=(kc == 7))
                                return ins
                            S.op("pe", mm, reads=[bwi, bhT], writes=bpp)
                            blk = NTB * I + tb
                            S.op("act", lambda e, pp=pp, blk=blk: e.activation(out=V1[:, blk, :, 0:64], in_=pp[:, 0:384].rearrange("p (h d) -> p h d", h=6), func=AF.Copy),
                                 reads=bpp, writes=[bV1])
                    if t == 5:
                        for tb in range(NTB):
                            pp, bpp = self.ps_half()

                            def mm(e, wi=wi, tb=tb, pp=pp):
                                ins = None
                                for kc in range(8):
                                    ins = e.matmul(out=pp, lhsT=hT[:, kc, tb * 128:(tb + 1) * 128], rhs=wi[:, kc, :], start=(kc == 0), stop=(kc == 7))
                                return ins
                            S.op("pe", mm, reads=[bwi, bhT], writes=bpp)
                            S.op("act", lambda e, pp=pp, tb=tb: e.activation(out=ztok[:, tb, :], in_=pp[:, 0:384], func=AF.Copy), reads=bpp, writes=[bztok])
                            S.op("dve", lambda e, pp=pp, tb=tb: e.tensor_copy(out=smt[:, tb, :], in_=pp[:, 384:402]), reads=bpp, writes=[bsmt])
                if pend is not None:
                    for _ in pend:
                        pass
                    pend = None
                if 'C' in _st:
                  self._scalars(I, smt, bsmt, sm, aneg, bdrv, dtb, fbias, bsp, c_all, bcall, carry, bcarry, bias_tab, bbias)
                if 'D' not in _st:
                    continue
                S.op("dve", lambda e: e.tensor_copy(out=qal[:, 0:3, :], in_=dnT[0:64, 0:3, :]), reads=bdnT[0:3], writes=[bqal])
                S.op("dve", lambda e: e.tensor_copy(out=qal[:, 3:6, :], in_=dnT[64:128, 0:3, :]), reads=bdnT[0:3] + [bqal], writes=[bqal])
                S.op("dve", lambda e: e.tensor_copy(out=kal[:, 0:3, :], in_=dnT[0:64, 3:6, :]), reads=bdnT[3:6], writes=[bkal])
                S.op("dve", lambda e: e.tensor_copy(out=kal[:, 3:6, :], in_=dnT[64:128, 3:6, :]), reads=bdnT[3:6] + [bkal], writes=[bkal])
                for tb in range(NTB):
                    for which, dst, bdst in ((3, ktok, bktok), (6, vtok, bvtok)):
                        pT, bpT = self.ps_half()
                        pTb = pT.bitcast(BF16)

                        def tr(e, tb=tb, which=which, pTb=pTb):
                            ins = None
                            for c in range(3):
                                ins = e.transpose(out=pTb[:, c * 128:(c + 1) * 128], in_=dnT[:, which + c, tb * 128:(tb + 1) * 128], identity=ident_b)
                            return ins
                        S.op("pe", tr, reads=[bdnT[which], bdnT[which + 1], bdnT[which + 2]] + CB, writes=bpT)
                        S.op("act", lambda e, dst=dst, tb=tb, pTb=pTb: e.activation(out=dst[:, tb, :], in_=pTb[:, 0:384], func=AF.Copy), reads=bpT, writes=[bdst])
                def dn_all(I=I):
                    for tb in range(NTB if 'E' in _st else 0):
                        yield from self._dn_chunk(I, tb, dnT, bdnT, qal, bqal, kal, bkal, ktok, bktok, vtok, bvtok, ztok, bztok, sm, W, S32, bS32, Sb, bSb, id6, bid6,
                                                  onorm, bsp, mixT, bmix)

                def fx_all(I=I):
                    for h in range(6 if 'F' in _st else 0):
                        yield from self._fox_head(I, h, fxq, bfxq, KT, bKT, V1, bV1, bias_tab, bbias, Es, rr, brr, mixT, bmix)
                n_dn = NTB * 26
                n_fx = 6 * (NTB * I + NTB + 1)
                g1, g2 = dn_all(), fx_all()
                d1 = d2 = 0
                a1 = a2 = True
                while a1 or a2:
                    if a1 and (not a2 or d1 * n_fx <= d2 * n_dn):
                        try:
                            next(g1); d1 += 1
                        except StopIteration:
                            a1 = False
                    else:
                        try:
                            next(g2); d2 += 1
                        except StopIteration:
                            a2 = False
                for tb in range(NTB if 'G' in _st else 0):
                    xt, bxt = xts[tb % 2]
                    S.dma("sp", xt[:], xin[t0 + tb * 128:t0 + (tb + 1) * 128, :], reads=[bxin], writes=[bxt])
                    xo, bxo = x1t[tb % 2]
                    for nh in range(2):
                        pp, bpp = self.ps_half()

                        def mm(e, tb=tb, nh=nh, pp=pp):
                            ins = None
                            for c in range(8):
                                ins = e.matmul(out=pp, lhsT=mixT[:, c, tb * 128:(tb + 1) * 128], rhs=wo[:, c, nh * 512:(nh + 1) * 512], start=(c == 0), stop=(c == 7))
                            return ins
                        S.op("pe", mm, reads=bmix + [bwo], writes=bpp)
                        S.op("dve", lambda e, xo=xo, xt=xt, nh=nh, pp=pp: e.tensor_tensor(out=xo[:, nh * 512:(nh + 1) * 512], in0=pp, in1=xt[:, nh * 512:(nh + 1) * 512], op=ALU.add),
                             reads=bpp + [bxt], writes=[bxo])
                    S.dma("pool", x1[t0 + tb * 128:t0 + (tb + 1) * 128, :], xo[:], reads=[bxo], writes=[bx1], add=True)
                if I == 0:
                    self._cast_for_ffn(l)
            S.barrier()
            with nc.named_scope(f"mixer{l}"):
                S.emit()

    def _dn_conv(self, fc, pp, bpp, cin_, cacc_, sqb_, rsd_, dhalo, bdhalo, convw, bsp, dnT, bdnT):
        S = self.S
        cin, bcin = cin_; acc, bacc = cacc_; sq, bsq = sqb_; rs, brs = rsd_
        S.op("act", lambda e: e.activation(out=cin[:, 3:3 + SBM], in_=pp, func=AF.Copy), reads=bpp, writes=[bcin])
        S.op("pool", lambda e: e.tensor_copy(out=cin[:, 0:3], in_=dhalo[:, fc, :]), reads=[bdhalo, bcin], writes=[bcin])
        S.op("pool", lambda e: e.tensor_copy(out=dhalo[:, fc, :], in_=cin[:, SBM:SBM + 3]), reads=[bcin], writes=[bdhalo])
        S.op("dve", lambda e: e.tensor_scalar(out=acc[:], in0=cin[:, 3:3 + SBM], scalar1=convw[:, fc, 3:4], scalar2=None, op0=ALU.mult),
             reads=[bcin, bsp], writes=[bacc])
        for j in range(3):
            eng = "dve"
            S.op(eng, lambda e, j=j: e.scalar_tensor_tensor(out=acc[:], in0=cin[:, j:j + SBM], scalar=convw[:, fc, j:j + 1], in1=acc[:], op0=ALU.mult, op1=ALU.add),
                 reads=[bcin, bsp, bacc], writes=[bacc])
        if fc >= 6:
            S.op("act", lambda e: e.activation(out=dnT[:, fc, :], in_=acc[:], func=AF.Silu), reads=[bacc], writes=[bdnT[fc]])
            return
        S.op("act", lambda e: e.activation(out=acc[:], in_=acc[:], func=AF.Silu), reads=[bacc], writes=[bacc])
        S.op("pool", lambda e: e.tensor_tensor(out=sq[:], in0=acc[:], in1=acc[:], op=ALU.mult), reads=[bacc], writes=[bsq])
        p2_, bp2 = self.ps_half()
        p2 = p2_[:, 0:SBM]
        blk2_b = self.blk2_b
        S.op("pe", lambda e: e.matmul(out=p2, lhsT=blk2_b, rhs=sq[:], start=True, stop=True), reads=[bsq] + self.CB, writes=bp2)
        yield
        S.op("act", lambda e: e.activation(out=rs[:], in_=p2, func=AF.Sqrt, bias=EPS), reads=bp2, writes=[brs])
        S.op("dve", lambda e: e.reciprocal(out=rs[:], in_=rs[:]), reads=[brs], writes=[brs])
        scl = 0.125 if fc < 3 else 1.0
        S.op("dve", lambda e: e.scalar_tensor_tensor(out=dnT[:, fc, :], in0=acc[:], scalar=scl, in1=rs[:], op0=ALU.mult, op1=ALU.mult),
             reads=[bacc, brs], writes=[bdnT[fc]])

    def _fx_qk(self, c, I, pp, bpp, sqb_, rsd_, fxqg8, bdrv, fxkg, bsp, fxq, bfxq, KT, bKT):
        S = self.S
        sq, bsq = sqb_; rs, brs = rsd_
        S.op("act", lambda e: e.activation(out=sq[:], in_=pp, func=AF.Square), reads=bpp, writes=[bsq])
        p2_, bp2 = self.ps_half()
        p2 = p2_[:, 0:SBM]
        blk2_b = self.blk2_b
        S.op("pe", lambda e: e.matmul(out=p2, lhsT=blk2_b, rhs=sq[:], start=True, stop=True), reads=[bsq] + self.CB, writes=bp2)
        yield
        S.op("act", lambda e: e.activation(out=rs[:], in_=p2, func=AF.Sqrt, scale=1.0 / 64, bias=EPS), reads=bp2, writes=[brs])
        S.op("dve", lambda e: e.reciprocal(out=rs[:], in_=rs[:]), reads=[brs], writes=[brs])
        if c < 3:
            S.op("dve", lambda e: e.scalar_tensor_tensor(out=fxq[:, c, :], in0=pp, scalar=fxqg8, in1=rs[:], op0=ALU.mult, op1=ALU.mult),
                 reads=bpp + [bdrv, brs], writes=[bfxq[c]])
        else:
            S.op("dve", lambda e: e.scalar_tensor_tensor(out=KT[:, c - 3, I * SBM:(I + 1) * SBM], in0=pp, scalar=fxkg, in1=rs[:], op0=ALU.mult, op1=ALU.mult),
                 reads=bpp + [bsp, brs], writes=[bKT])

    def _pool_mix(self, c, I, pp, bpp, pin_, phalo, bphalo, ps2, bps2, ps4, bps4, ps8, bps8, pW, bpW, py, bpy, pwb, bpwb, pscale, bsp, mixT, bmix):
        S = self.S
        pin, bpin = pin_
        N = 16 + SBM
        S.op("act", lambda e: e.activation(out=pin[:, 16:N], in_=pp, func=AF.Copy), reads=bpp, writes=[bpin])
        S.op("pool", lambda e: e.tensor_copy(out=pin[:, 0:16], in_=phalo[:, c, :]), reads=[bphalo, bpin], writes=[bpin])
        S.op("pool", lambda e: e.tensor_copy(out=phalo[:, c, :], in_=pin[:, SBM:N]), reads=[bpin], writes=[bphalo])
        S.op("pool", lambda e: e.tensor_tensor(out=ps2[:, 2:N], in0=pin[:, 2:N], in1=pin[:, 1:N - 1], op=ALU.add), reads=[bpin], writes=[bps2])
        if c == 0:
            S.op("pool", lambda e: e.tensor_copy(out=pW[0:64, :], in_=ps2[0:64, 16:N]), reads=[bps2], writes=[bpW])
            S.op("pool", lambda e: e.tensor_tensor(out=pW[64:128, :], in0=ps2[64:128, 16:N], in1=ps2[64:128, 14:N - 2], op=ALU.add), reads=[bps2, bpW], writes=[bpW])
        else:
            S.op("pool", lambda e: e.tensor_tensor(out=ps4[:, 4:N], in0=ps2[:, 4:N], in1=ps2[:, 2:N - 2], op=ALU.add), reads=[bps2], writes=[bps4])
            S.op("pool", lambda e: e.tensor_tensor(out=ps8[:, 8:N], in0=ps4[:, 8:N], in1=ps4[:, 4:N - 4], op=ALU.add), reads=[bps4], writes=[bps8])
            S.op("pool", lambda e: e.tensor_copy(out=pW[0:64, :], in_=ps8[0:64, 16:N]), reads=[bps8], writes=[bpW])
            S.op("pool", lambda e: e.tensor_tensor(out=pW[64:128, :], in0=ps8[64:128, 16:N], in1=ps8[64:128, 8:N - 8], op=ALU.add), reads=[bps8, bpW], writes=[bpW])
        pinvw = self.pinvw
        S.op("dve", lambda e: e.scalar_tensor_tensor(out=py[:], in0=pW[:], scalar=pinvw[:, c:c + 1], in1=pin[:, 16:N], op0=ALU.mult, op1=ALU.subtract),
             reads=[bpW, bpin] + self.CB, writes=[bpy])
        if I == 0:
            pinvc = self.pinvc[c]
            S.op("dve", lambda e: e.tensor_tensor(out=pW[:, 0:16], in0=pW[:, 0:16], in1=pinvc, op=ALU.mult), reads=[bpW, bpy] + self.CB, writes=[bpW])
            S.op("dve", lambda e: e.tensor_tensor(out=py[:, 0:16], in0=pW[:, 0:16], in1=pin[:, 16:32], op=ALU.subtract), reads=[bpW, bpin, bpy], writes=[bpy])
        p2_, bp2 = self.ps_half()
        p2 = p2_[:, 0:SBM]
        S.op("pe", lambda e: e.matmul(out=p2, lhsT=pwb[:, c, :], rhs=py[:], start=True, stop=True), reads=[bpy, bpwb], writes=bp2)
        S.op("act", lambda e: e.activation(out=mixT[:, 6 + c, :], in_=p2, func=AF.Copy, scale=pscale[:, c:c + 1]), reads=bp2 + [bsp], writes=[bmix[6 + c]])
        return
        yield

    def _scalars(self, I, smt, bsmt, sm, aneg, bdrv, dtb, fbias, bsp, c_all, bcall, carry, bcarry, bias_tab, bbias):
        S = self.S
        g, bg = sm["g"]; beta, bbeta = sm["beta"]; logf, blogf = sm["logf"]; tt, btt = sm["t"]
        G, bG = sm["G"]; Gl, bGl = sm["Gl"]; eG, beG = sm["eG"]; ed, bed = sm["ed"]; lb, blb = sm["lb"]
        cum, bcum = sm["cum"]; tot, btot = sm["tot"]; cref, bcref = sm["cref"]
        NC = NTB * 6
        bc4 = lambda ap: ap.unsqueeze(1).to_broadcast([128, NTB, 6])
        S.op("dve", lambda e: e.tensor_tensor(out=tt[:], in0=smt[:, :, 0:6], in1=bc4(dtb), op=ALU.add), reads=[bsmt, bsp], writes=[btt])
        S.op("act", lambda e: e.activation(out=tt[:], in_=tt[:], func=AF.Exp), reads=[btt], writes=[btt])
        S.op("act", lambda e: e.activation(out=tt[:], in_=tt[:], func=AF.Ln, bias=1.0), reads=[btt], writes=[btt])
        S.op("dve", lambda e: e.tensor_tensor(out=g[:], in0=tt[:], in1=bc4(aneg), op=ALU.mult), reads=[btt, bdrv], writes=[bg])
        S.op("dve", lambda e: e.tensor_tensor(out=tt[:], in0=smt[:, :, 12:18], in1=bc4(fbias), op=ALU.add), reads=[bsmt, bsp, btt], writes=[btt])
        S.op("act", lambda e: e.activation(out=tt[:], in_=tt[:], func=AF.Exp, scale=-1.0), reads=[btt], writes=[btt])
        S.op("act", lambda e: e.activation(out=tt[:], in_=tt[:], func=AF.Ln, bias=1.0), reads=[btt], writes=[btt])
        S.op("dve", lambda e: e.tensor_scalar(out=logf[:], in0=tt[:], scalar1=-1.0, scalar2=None, op0=ALU.mult), reads=[btt], writes=[blogf])
        S.op("act", lambda e: e.activation(out=beta[:], in_=smt[:, :, 6:12], func=AF.Sigmoid), reads=[bsmt], writes=[bbeta])
        tri_f, ones_f = self.tri_f, self.ones_f
        fl = lambda t: t[:].rearrange("p a b -> p (a b)")
        p1, bp1 = self.ps_half()
        S.op("pe", lambda e: e.matmul(out=p1[:, 0:NC], lhsT=tri_f, rhs=fl(g), start=True, stop=True), reads=[bg] + self.CB, writes=bp1)
        S.op("dve", lambda e: e.tensor_copy(out=fl(G), in_=p1[:, 0:NC]), reads=bp1, writes=[bG])
        p2, bp2 = self.ps_half()
        S.op("pe", lambda e: e.matmul(out=p2[:, 0:NC], lhsT=ones_f, rhs=fl(g), start=True, stop=True), reads=[bg] + self.CB, writes=bp2)
        S.op("dve", lambda e: e.tensor_copy(out=fl(Gl), in_=p2[:, 0:NC]), reads=bp2, writes=[bGl])
        S.op("act", lambda e: e.activation(out=eG[:], in_=G[:], func=AF.Exp), reads=[bG], writes=[beG])
        S.op("act", lambda e: e.activation(out=lb[:], in_=Gl[:], func=AF.Exp), reads=[bGl], writes=[blb])
        S.op("dve", lambda e: e.tensor_tensor(out=ed[:], in0=Gl[:], in1=G[:], op=ALU.subtract), reads=[bGl, bG], writes=[bed])
        S.op("act", lambda e: e.activation(out=ed[:], in_=ed[:], func=AF.Exp), reads=[bed], writes=[bed])
        p3, bp3 = self.ps_half()
        S.op("pe", lambda e: e.matmul(out=p3[:, 0:NC], lhsT=tri_f, rhs=fl(logf), start=True, stop=True), reads=[blogf] + self.CB, writes=bp3)
        S.op("dve", lambda e: e.tensor_copy(out=fl(cum), in_=p3[:, 0:NC]), reads=bp3, writes=[bcum])
        p4, bp4 = self.ps_half()
        S.op("pe", lambda e: e.matmul(out=p4[:, 0:NC], lhsT=ones_f, rhs=fl(logf), start=True, stop=True), reads=[blogf] + self.CB, writes=bp4)
        S.op("dve", lambda e: e.tensor_copy(out=fl(tot), in_=p4[:, 0:NC]), reads=bp4, writes=[btot])
        for tb in range(NTB):
            blk = NTB * I + tb
            S.op("dve", lambda e, tb=tb, blk=blk: e.tensor_tensor(out=c_all[:, blk, :], in0=cum[:, tb, :], in1=carry[:], op=ALU.add),
                 reads=[bcum, bcarry], writes=[bcall])
            S.op("dve", lambda e, tb=tb: e.tensor_tensor(out=carry[:], in0=carry[:], in1=tot[:, tb, :], op=ALU.add),
                 reads=[btot, bcarry], writes=[bcarry])
            if tb == NTB // 2 - 1:
                S.op("dve", lambda e: e.tensor_copy(out=cref[:, 0, :], in_=carry[:]), reads=[bcarry], writes=[bcref])
        nb = NTB * I + NTB
        S.op("dve", lambda e: e.tensor_tensor(out=bias_tab[:, 0:nb, :], in0=cref[:, 0, :].unsqueeze(1).to_broadcast([128, nb, 6]), in1=c_all[:, 0:nb, :], op=ALU.subtract),
             reads=[bcref, bcall], writes=[bbias])

    def _dn_chunk(self, I, tb, dnT, bdnT, qal, bqal, kal, bkal, ktok, bktok, vtok, bvtok, ztok, bztok, sm, W, S32, bS32, Sb, bSb, id6, bid6, onorm, bsp, mixT, bmix):
        S = self.S
        CB = self.CB
        g, bg = sm["g"]; beta, bbeta = sm["beta"]; G, bG = sm["G"]; eG, beG = sm["eG"]; ed, bed = sm["ed"]; lb, blb = sm["lb"]
        ts = slice(tb * 128, (tb + 1) * 128)
        hs = lambda h: slice((h % 2) * 64, (h % 2) * 64 + 64)
        qT = lambda h: dnT[hs(h), h // 2, ts]
        kT = lambda h: dnT[hs(h), 3 + h // 2, ts]
        bq = bdnT[0:3]; bk = bdnT[3:6]
        v6 = lambda ap: ap.rearrange("p (h d) -> p h d", h=6)
        bc128 = lambda ap: ap.unsqueeze(2).to_broadcast([128, 6, 128])
        bc64 = lambda ap: ap.unsqueeze(2).to_broadcast([128, 6, 64])
        tri_f, ident_b, ident_f = self.tri_f, self.ident_b, self.ident_f
        diff, bdiff = W["diff"]; es, bes = W["es"]; ec, bec = W["ec"]
        Bp, bBp = W["Bp"]; Ap, bAp = W["Ap"]; qkT, bqkT = W["qkT"]; Xn, bXn = W["Xn"]
        kg, bkg = W["kg"]; kd, bkd = W["kd"]; ut, but = W["ut"]; wT, bwT = W["wT"]
        vn, bvn = W["vn"]; o, bo = W["o"]; osq, bosq = W["osq"]; oss, boss = W["oss"]; ors, bors = W["ors"]
        zs, bzs = W["zs"]; of, bof = W["of"]
        HS = (slice(0, 4), slice(4, 6))

        def two(eng, fn, reads, writes):
            S.op(eng, lambda e: fn(e, slice(0, 6)), reads=reads, writes=writes)
        bc128s = lambda ap, hsl: ap[:, hsl].unsqueeze(2).to_broadcast([128, hsl.stop - hsl.start, 128])
        mk6 = lambda m, hsl: m.unsqueeze(1).to_broadcast([128, hsl.stop - hsl.start, 128])
        pG, bpG = self.ps_full("dn")
        pG6 = pG[:, 0:768].rearrange("p (h i) -> p h i", h=6)

        def mmG(e):
            ins = None
            for h in range(6):
                ins = e.matmul(out=pG6[:, h, :], lhsT=g[:, tb, h:h + 1].to_broadcast([128, 128]), rhs=tri_f, start=True, stop=True)
            return ins
        S.op("pe", mmG, reads=[bg] + CB, writes=bpG)
        two("dve", lambda e, hsl: e.tensor_tensor(out=diff[:, hsl, :], in0=pG6[:, hsl, :], in1=bc128s(G[:, tb, :], hsl), op=ALU.subtract), bpG + [bG], [bdiff])
        negstrict = self.negstrict
        S.op("dve", lambda e: e.scalar_tensor_tensor(out=diff[:], in0=diff[:], scalar=0.0, in1=negstrict.unsqueeze(1).to_broadcast([128, 6, 128]), op0=ALU.min, op1=ALU.add),
             reads=[bdiff] + CB, writes=[bdiff])
        S.op("act", lambda e: e.activation(out=es[:], in_=diff[:], func=AF.Exp), reads=[bdiff], writes=[bes])
        S.op("dve", lambda e: e.tensor_tensor(out=ec[:], in0=es[:], in1=ident_b.unsqueeze(1).to_broadcast([128, 6, 128]), op=ALU.add), reads=[bes] + CB, writes=[bec])
        if int(os.environ.get('MK_E1', 9)) < 2:
            return
        yield
        pK, bpK = self.ps_full("dn")
        pK6 = pK[:, 0:768].rearrange("p (h i) -> p h i", h=6)

        def mmK(e):
            ins = None
            for h in range(6):
                ins = e.matmul(out=pK6[:, h, :], lhsT=kal[:, (h % 2) * 3 + h // 2, ts], rhs=kal[:, (h % 2) * 3 + h // 2, ts], start=True, stop=True)
            return ins
        S.op("pe", mmK, reads=[bkal], writes=bpK)
        two("dve", lambda e, hsl: e.tensor_tensor(out=diff[:, hsl, :], in0=pK6[:, hsl, :], in1=es[:, hsl, :], op=ALU.mult), bpK + [bes, bdiff], [bdiff])
        S.op("dve", lambda e: e.tensor_tensor(out=Bp[:], in0=diff[:], in1=bc128(beta[:, tb, :]), op=ALU.mult), reads=[bdiff, bbeta], writes=[bBp])
        yield
        pQ, bpQ = self.ps_full("dn")
        pQ6 = pQ[:, 0:768].rearrange("p (h i) -> p h i", h=6)

        def mmQ(e):
            ins = None
            for h in range(6):
                ins = e.matmul(out=pQ6[:, h, :], lhsT=kal[:, (h % 2) * 3 + h // 2, ts], rhs=qal[:, (h % 2) * 3 + h // 2, ts], start=True, stop=True)
            return ins
        S.op("pe", mmQ, reads=[bkal, bqal], writes=bpQ)
        two("dve", lambda e, hsl: e.tensor_tensor(out=qkT[:, hsl, :], in0=pQ6[:, hsl, :], in1=ec[:, hsl, :], op=ALU.mult), bpQ + [bec], [bqkT])
        if int(os.environ.get('MK_E1', 9)) < 3:
            return
        yield
        pA, bpA = self.ps_half("dn")
        pA6 = pA.bitcast(BF16)[:, 0:768].rearrange("p (h i) -> p h i", h=6)

        def trA(e):
            ins = None
            for h in range(6):
                ins = e.transpose(out=pA6[:, h, :], in_=Bp[:, h, :], identity=ident_b)
            return ins
        S.op("pe", trA, reads=[bBp] + CB, writes=bpA)
        S.op("act", lambda e: e.activation(out=Ap[:], in_=pA6, func=AF.Copy), reads=bpA, writes=[bAp])
        if int(os.environ.get('MK_E', 9)) < 2:
            return
        yield
        U, bU = id6, bid6
        Tm, bT = id6, bid6
        Us = [W["Ua"], W["Ub"]]; Ts = [W["Ta"], W["Tb"]]
        negM0, negM0T = self.negM_b[0], self.negM0T_b
        Ua0, bUa0 = Us[0]
        Ta0, bTa0 = Ts[0]
        S.op("dve", lambda e: e.tensor_tensor(out=Xn[:], in0=Bp[:], in1=negM0.unsqueeze(1).to_broadcast([128, 6, 128]), op=ALU.mult), reads=[bBp] + CB, writes=[bXn])
        S.op("pool", lambda e: e.tensor_tensor(out=Ua0[:], in0=Xn[:], in1=id6[:], op=ALU.add), reads=[bXn, bid6], writes=[bUa0])
        S.op("dve", lambda e: e.tensor_tensor(out=Ta0[:], in0=Ap[:], in1=negM0T.unsqueeze(1).to_broadcast([128, 6, 128]), op=ALU.mult), reads=[bAp] + CB, writes=[bTa0])
        S.op("pool", lambda e: e.tensor_tensor(out=Ta0[:], in0=Ta0[:], in1=id6[:], op=ALU.add), reads=[bTa0, bid6], writes=[bTa0])
        U, bU = Ua0, bUa0
        Tm, bT = Ta0, bTa0
        yield
        for lv in range(1, 7):
            pX, bpX = self.ps_full("dn")
            pX6 = pX[:, 0:768].rearrange("p (h i) -> p h i", h=6)

            def mmX(e, U=U, pX6=pX6):
                ins = None
                for h in range(6):
                    ins = e.matmul(out=pX6[:, h, :], lhsT=Ap[:, h, :], rhs=U[:, h, :], start=True, stop=True)
                return ins
            S.op("pe", mmX, reads=[bAp, bU], writes=bpX)
            negM = self.negM_b[lv]
            two("dve", lambda e, hsl, pX6=pX6, negM=negM: e.tensor_tensor(out=Xn[:, hsl, :], in0=pX6[:, hsl, :], in1=mk6(negM, hsl), op=ALU.mult),
                bpX + CB, [bXn])
            yield
            Un, bUn = Us[lv % 2]
            pU, bpU = self.ps_full("dn")
            pU6 = pU[:, 0:768].rearrange("p (h i) -> p h i", h=6)

            def mmU(e, U=U, Tm=Tm, pU6=pU6):
                ins = None
                for h in range(6):
                    e.matmul(out=pU6[:, h, :], lhsT=ident_b, rhs=U[:, h, :], start=True, stop=False)
                    ins = e.matmul(out=pU6[:, h, :], lhsT=Tm[:, h, :], rhs=Xn[:, h, :], start=False, stop=True)
                return ins
            S.op("pe", mmU, reads=[bU, bT, bXn] + CB, writes=bpU)
            two("act", lambda e, hsl, Un=Un, pU6=pU6: e.activation(out=Un[:, hsl, :], in_=pU6[:, hsl, :], func=AF.Copy), bpU, [bUn])
            if lv < 6:
                Tn, bTn = Ts[lv % 2]
                pT, bpT = self.ps_full("dn")
                pT6 = pT[:, 0:768].rearrange("p (h i) -> p h i", h=6)

                def mmT(e, Tm=Tm, pT6=pT6):
                    ins = None
                    for h in range(6):
                        e.matmul(out=pT6[:, h, :], lhsT=ident_b, rhs=Tm[:, h, :], start=True, stop=False)
                        ins = e.matmul(out=pT6[:, h, :], lhsT=Xn[:, h, :], rhs=Tm[:, h, :], start=False, stop=True)
                    return ins
                S.op("pe", mmT, reads=[bT, bXn] + CB, writes=bpT)
                two("dve", lambda e, hsl, Tn=Tn, pT6=pT6: e.tensor_copy(out=Tn[:, hsl, :], in_=pT6[:, hsl, :]), bpT, [bTn])
                Tm, bT = Tn, bTn
            U, bU = Un, bUn
            yield
        if int(os.environ.get('MK_E', 9)) < 3:
            return
        yield
        S.op("dve", lambda e: e.tensor_tensor(out=kg[:], in0=v6(ktok[:, tb, :]), in1=bc64(eG[:, tb, :]), op=ALU.mult), reads=[bktok, beG], writes=[bkg])
        S.op("dve", lambda e: e.tensor_tensor(out=kd[:], in0=v6(ktok[:, tb, :]), in1=bc64(ed[:, tb, :]), op=ALU.mult), reads=[bktok, bed], writes=[bkd])
        pu, bpu = self.ps_half("dn")
        pu6 = pu[:, 0:384].rearrange("p (h d) -> p h d", h=6)

        def mmu(e, U=U):
            ins = None
            for h in range(6):
                ins = e.matmul(out=pu6[:, h, :], lhsT=U[:, h, :], rhs=vtok[:, tb, h * 64:(h + 1) * 64], start=True, stop=True)
            return ins
        S.op("pe", mmu, reads=[bU, bvtok], writes=bpu)
        S.op("act", lambda e: e.activation(out=ut[:], in_=pu6, func=AF.Copy), reads=bpu, writes=[but])
        yield
        pw_, bpw_ = self.ps_full("dn")
        pw6 = pw_[0:64, 0:768].rearrange("p (h i) -> p h i", h=6)

        def mmw(e, U=U):
            ins = None
            for h in range(6):
                ins = e.matmul(out=pw6[:, h, :], lhsT=kg[:, h, :], rhs=U[:, h, :], start=True, stop=True)
            return ins
        S.op("pe", mmw, reads=[bU, bkg], writes=bpw_)
        two("act", lambda e, hsl: e.activation(out=wT[:, hsl, :], in_=pw6[:, hsl, :], func=AF.Copy), bpw_, [bwT])
        yield
        p1, bp1 = self.ps_half("dn")
        p16 = p1[:, 0:384].rearrange("p (h d) -> p h d", h=6)

        def mm1(e):
            ins = None
            for h in range(6):
                ins = e.matmul(out=p16[:, h, :], lhsT=wT[:, h, :], rhs=Sb[:, h, :], start=True, stop=True)
            return ins
        S.op("pe", mm1, reads=[bwT, bSb], writes=bp1)
        S.op("dve", lambda e: e.tensor_tensor(out=ut[:], in0=ut[:], in1=p16, op=ALU.subtract), reads=bp1 + [but], writes=[but])
        S.op("dve", lambda e: e.tensor_tensor(out=vn[:], in0=ut[:], in1=bc64(beta[:, tb, :]), op=ALU.mult), reads=[but, bbeta], writes=[bvn])
        yield
        p2a, bp2a = self.ps_half("dn")
        p2a6 = p2a[:, 0:384].rearrange("p (h d) -> p h d", h=6)

        def mm2a(e):
            ins = None
            for h in range(6):
                ins = e.matmul(out=p2a6[:, h, :], lhsT=qal[:, (h % 2) * 3 + h // 2, ts], rhs=Sb[:, h, :], start=True, stop=True)
            return ins
        S.op("pe", mm2a, reads=[bqal, bSb], writes=bp2a)
        p2b, bp2b = self.ps_half("dn")
        p2b6 = p2b[:, 0:384].rearrange("p (h d) -> p h d", h=6)

        def mm2b(e):
            ins = None
            for h in range(6):
                ins = e.matmul(out=p2b6[:, h, :], lhsT=qkT[:, h, :], rhs=vn[:, h, :], start=True, stop=True)
            return ins
        S.op("pe", mm2b, reads=[bqkT, bvn], writes=bp2b)
        p3, bp3 = self.ps_half("dn")
        p36 = p3[0:64, 0:384].rearrange("p (h d) -> p h d", h=6)

        def mm3(e):
            ins = None
            for h in range(6):
                ins = e.matmul(out=p36[:, h, :], lhsT=kd[:, h, :], rhs=vn[:, h, :], start=True, stop=True)
            return ins
        S.op("pe", mm3, reads=[bkd, bvn], writes=bp3)
        S.op("dve", lambda e: e.tensor_tensor(out=S32[:], in0=S32[:], in1=lb[0:64, tb, :].unsqueeze(2).to_broadcast([64, 6, 64]), op=ALU.mult),
             reads=[bS32, blb], writes=[bS32])
        S.op("dve", lambda e: e.tensor_tensor(out=S32[:], in0=S32[:], in1=p36, op=ALU.add), reads=[bS32] + bp3, writes=[bS32])
        S.op("act", lambda e: e.activation(out=Sb[:], in_=S32[:], func=AF.Copy), reads=[bS32], writes=[bSb])
        yield
        S.op("dve", lambda e: e.tensor_tensor(out=o[:], in0=p2a6, in1=bc64(eG[:, tb, :]), op=ALU.mult), reads=bp2a + [beG], writes=[bo])
        S.op("dve", lambda e: e.tensor_tensor(out=o[:], in0=o[:], in1=p2b6, op=ALU.add), reads=[bo] + bp2b, writes=[bo])
        S.op("pool", lambda e: e.tensor_tensor(out=osq[:], in0=o[:], in1=o[:], op=ALU.mult), reads=[bo], writes=[bosq])
        S.op("dve", lambda e: e.tensor_reduce(out=oss[:], in_=osq[:], axis=AX.X, op=ALU.add), reads=[bosq], writes=[boss])
        S.op("act", lambda e: e.activation(out=ors[:], in_=oss[:], func=AF.Sqrt, scale=1.0 / 64, bias=EPS), reads=[boss], writes=[bors])
        S.op("dve", lambda e: e.reciprocal(out=ors[:], in_=ors[:]), reads=[bors], writes=[bors])
        S.op("dve", lambda e: e.tensor_tensor(out=o[:], in0=o[:], in1=bc64(ors[:]), op=ALU.mult), reads=[bo, bors], writes=[bo])
        S.op("dve", lambda e: e.tensor_tensor(out=o[:], in0=o[:], in1=onorm.unsqueeze(1).to_broadcast([128, 6, 64]), op=ALU.mult), reads=[bo, bsp], writes=[bo])
        S.op("act", lambda e: e.activation(out=zs[:], in_=ztok[:, tb, :], func=AF.Silu), reads=[bztok], writes=[bzs])
        S.op("dve", lambda e: e.tensor_tensor(out=of[:], in0=o[:].rearrange("p h d -> p (h d)"), in1=zs[:], op=ALU.mult), reads=[bo, bzs], writes=[bof])
        yield
        pO, bpO = self.ps_half("dn")
        pOb = pO.bitcast(BF16)

        def trO(e):
            ins = None
            for c in range(3):
                ins = e.transpose(out=pOb[:, c * 128:(c + 1) * 128], in_=of[:, c * 128:(c + 1) * 128], identity=ident_b)
            return ins
        S.op("pe", trO, reads=[bof] + CB, writes=bpO)
        S.op("act", lambda e: e.activation(out=mixT[:, 0:3, ts], in_=pOb[:, 0:384].rearrange("p (c t) -> p c t", c=3), func=AF.Copy),
             reads=bpO, writes=bmix[0:3])
        yield

    def _fox_head(self, I, h, fxq, bfxq, KT, bKT, V1, bV1, bias_tab, bbias, Es, rr, brr, mixT, bmix):
        S = self.S
        nj = NTB * I + NTB
        ones_f = self.ones_f
        c = h // 2
        hp = slice((h % 2) * 64, (h % 2) * 64 + 64)
        po_, bpo = self.ps_resv()
        po = po_[:, 0:SBM]

        def score(j):
            ps__, bps_ = self.ps_half("fx")
            ps_ = ps__[:, 0:SBM]
            S.op("pe", lambda e: e.matmul(out=ps_, lhsT=KT[hp, c, j * 128:(j + 1) * 128], rhs=fxq[hp, c, :], start=True, stop=True),
                 reads=[bKT, bfxq[c]], writes=bps_)
            return ps_, bps_

        def pv(j, E, bE):
            S.op("pe", lambda e: e.matmul(out=po[0:65, :], lhsT=V1[:, j, h, :], rhs=E[:], start=(j == 0), stop=(j == nj - 1)),
                 reads=[bE, bV1], writes=bpo)

        def expo(j, E, bE, ps_, bps_):
            S.op("act", lambda e: e.activation(out=E[:], in_=ps_, func=AF.Exp, bias=bias_tab[:, j, h:h + 1]), reads=bps_ + [bbias], writes=[bE])
            if j >= NTB * I:
                dm = self.dmask_b[j - NTB * I]
                S.op("pool", lambda e: e.tensor_tensor(out=E[:], in0=E[:], in1=dm, op=ALU.mult), reads=[bE] + self.CB, writes=[bE])

        nxt = score(0)
        for j in range(nj):
            ps_, bps_ = nxt
            E, bE = Es[(h * 64 + j) % 3]
            expo(j, E, bE, ps_, bps_)
            if j + 1 < nj:
                nxt = score(j + 1)
            pv(j, E, bE)
            yield
        S.op("dve", lambda e: e.reciprocal(out=rr[64:65, :], in_=po[64:65, :]), reads=bpo + [brr], writes=[brr])
        pr_, bpr = self.ps_half("fx")
        pr = pr_[:, 0:SBM]
        S.op("pe", lambda e: e.matmul(out=pr[0:64, :], lhsT=ones_f[64:65, 0:64], rhs=rr[64:65, :], start=True, stop=True),
             reads=[brr] + self.CB, writes=bpr)
        S.op("act", lambda e: e.activation(out=rr[0:64, :], in_=pr[0:64, :], func=AF.Copy), reads=bpr + [brr], writes=[brr])
        S.op("dve", lambda e: e.tensor_tensor(out=mixT[hp, 3 + c, :], in0=po[0:64, :], in1=rr[0:64, :], op=ALU.mult), reads=bpo + [brr], writes=[bmix[3 + c]])
        yield

    def _phase_ffn(self, l, x1, bx1, xout, bxout):
        nc, S = self.nc, self.S
        CB = self.CB
        cast_todo = self._cast_for_next_layer(self.layers.index(l))
        n_per = -(-len(cast_todo) // 7) if cast_todo else 0
        moe = (l % 2 == 1)
        with contextlib.ExitStack() as ph:
            sb = lambda name, shape, dt: self.sb(ph, name, shape, dt)
            spt = sb("fspt", [128, SP_COLS], F32); bsp = Buf()
            S.dma("sp", spt[:], self.d_sp[l * 128:(l + 1) * 128, :], writes=[bsp])
            g2T = spt[:, 8:16]
            acc = sb("acc", [128, 4, D], F32); bacc = [Buf() for _ in range(4)]
            nwork = (None, None, sb("fn_ss", [128, 1], F32), Buf(), sb("fn_rs", [128, 1], F32), Buf(),
                     sb("fn_xn", [128, D], BF16), Buf())
            h2T = sb("h2T", [128, 8, SB], BF16); bh2T = Buf()
            wgus = [(sb(f"wgu{i}", [128, 2, 8, 256], BF16), Buf()) for i in range(2)]
            nfd = 12 if moe else 11
            wds = [(sb(f"wd{i}", [128, nfd, D], BF16), Buf()) for i in range(2)]
            nact = 12 if moe else 22
            actT = sb("actT", [128, nact, SB], BF16); bact = [Buf() for _ in range(nact)]
            sgs = [(sb(f"sg{i}", [128, SB], F32), Buf()) for i in range(2)]
            if moe:
                rt = sb("rt", [128, 8, 8], F32); brt = Buf()
                S.dma("sp", rt[:].rearrange("p a b -> p (a b)"), self.d_router, writes=[brt])
                h2Tf = sb("h2Tf", [128, 8, 128], F32); bh2Tf = Buf()
                xf = sb("xf", [128, D], F32); bxf = Buf()
                lg = sb("lg", [128, 4, 8], F32); blg = Buf()
                gates = sb("gates", [128, 4, 8], F32); bgates = Buf()
                gw = {k: (sb("gw_" + k, [128, 4, 8], F32), Buf()) for k in ("m1", "l2", "m2")}
                gs = {k: (sb("gs_" + k, [128, 4], F32), Buf()) for k in ("m1", "m2", "s", "s2")}
            wdi = 0
            wgi = 0
            for I in range(NSB):
                t0 = I * SB
                for tb in range(4):
                    S.dma("sp", acc[:, tb, :], x1[t0 + tb * 128:t0 + (tb + 1) * 128, :], reads=[bx1], writes=[bacc[tb]])
                    want = (h2Tf[:], bh2Tf, xf, bxf) if moe else None
                    self._norm_T(acc[:, tb, :], bacc[tb], g2T, bsp, h2T[:, :, tb * 128:(tb + 1) * 128], bh2T, nwork, want_f32=want)
                    if moe:
                        pl, bpl = self.ps_half()

                        def mml(e, pl=pl):
                            ins = None
                            for kc in range(8):
                                ins = e.matmul(out=pl[:, 0:8], lhsT=h2Tf[:, kc, :], rhs=rt[:, kc, :], start=(kc == 0), stop=(kc == 7))
                            return ins
                        S.op("pe", mml, reads=[bh2Tf, brt], writes=bpl)
                        S.op("dve", lambda e, pl=pl, tb=tb: e.tensor_copy(out=lg[:, tb, :], in_=pl[:, 0:8]), reads=bpl, writes=[blg])
                if moe:
                    self._gates(lg, blg, gates, bgates, gw, gs)
                    if self.debug:
                        for tb in range(4):
                            S.dma("pool", self.dbg_g[t0 + tb * 128:t0 + (tb + 1) * 128, 0:8], lg[:, tb, :], reads=[blg])
                            S.dma("pool", self.dbg_g[t0 + tb * 128:t0 + (tb + 1) * 128, 8:16], gates[:, tb, :], reads=[bgates])
                nexp = 8 if moe else 1
                for ex in range(nexp):
                    ngt = 6 if moe else 11
                    for gt in range(ngt):
                        wgu, bwgu = wgus[wgi % 2]; wgi += 1
                        if moe:
                            row = (ex * 6 + gt) * 128
                            S.dma("sp", wgu[:].rearrange("p a b c -> p (a b c)"), self.b_mgu[row:row + 128, :], reads=[self.wbuf[("mgu", row)]], writes=[bwgu])
                        else:
                            row = gt * 128
                            S.dma("sp", wgu[:].rearrange("p a b c -> p (a b c)"), self.b_wgu[row:row + 128, :], reads=[self.wbuf[("wgu", row)]], writes=[bwgu])
                        for j in range(2):
                            fc = 2 * gt + j
                            pg, bpg = self.ps_half()
                            pu, bpu = self.ps_half()

                            def mmg(e, wgu=wgu, j=j, pg=pg, which=0):
                                ins = None
                                for kc in range(8):
                                    ins = e.matmul(out=pg, lhsT=wgu[:, which, kc, j * 128:(j + 1) * 128], rhs=h2T[:, kc, :], start=(kc == 0), stop=(kc == 7))
                                return ins
                            S.op("pe", mmg, reads=[bwgu, bh2T], writes=bpg)
                            S.op("pe", lambda e, wgu=wgu, j=j, pu=pu: mmg(e, wgu, j, pu, 1), reads=[bwgu, bh2T], writes=bpu)
                            sg, bsg = sgs[fc % 2]
                            S.op("act", lambda e, sg=sg, pg=pg: e.activation(out=sg[:], in_=pg, func=AF.Silu), reads=bpg, writes=[bsg])
                            S.op("dve", lambda e, sg=sg, pu=pu, fc=fc: e.tensor_tensor(out=actT[:, fc, :], in0=sg[:], in1=pu, op=ALU.mult),
                                 reads=[bsg] + bpu, writes=[bact[fc]])
                    ndt = 1 if moe else 2
                    for dti in range(ndt):
                        wd, bwd = wds[wdi % 2]; wdi += 1
                        if moe:
                            S.dma("sp", wd[:].rearrange("p a b -> p (a b)"), self.b_md[ex * 128:(ex + 1) * 128, :], reads=[self.wbuf[("md", ex * 128)]], writes=[bwd])
                        else:
                            S.dma("sp", wd[:].rearrange("p a b -> p (a b)"), self.b_wd[dti * 128:(dti + 1) * 128, :], reads=[self.wbuf[("wd", dti * 128)]], writes=[bwd])
                        for tb in range(4):
                            for nh in range(2):
                                pd, bpd = self.ps_half()

                                def mmd(e, wd=wd, tb=tb, nh=nh, pd=pd, dti=dti):
                                    ins = None
                                    for f in range(nfd):
                                        ins = e.matmul(out=pd, lhsT=actT[:, dti * nfd + f, tb * 128:(tb + 1) * 128], rhs=wd[:, f, nh * 512:(nh + 1) * 512],
                                                       start=(f == 0), stop=(f == nfd - 1))
                                    return ins
                                S.op("pe", mmd, reads=[bwd] + bact[dti * nfd:(dti + 1) * nfd], writes=bpd)
                                asl = acc[:, tb, nh * 512:(nh + 1) * 512]
                                if moe:
                                    S.op("dve", lambda e, asl=asl, pd=pd, tb=tb, ex=ex: e.scalar_tensor_tensor(out=asl, in0=pd, scalar=gates[:, tb, ex:ex + 1], in1=asl, op0=ALU.mult, op1=ALU.add),
                                         reads=bpd + [bgates, bacc[tb]], writes=[bacc[tb]])
                                else:
                                    S.op("dve", lambda e, asl=asl, pd=pd: e.tensor_tensor(out=asl, in0=asl, in1=pd, op=ALU.add), reads=bpd + [bacc[tb]], writes=[bacc[tb]])
                for tb in range(4):
                    S.dma("pool", xout[t0 + tb * 128:t0 + (tb + 1) * 128, :], acc[:, tb, :], reads=[bacc[tb]], writes=[bxout], add=True)
                for _ in range(n_per):
                    if cast_todo:
                        self._cast(*cast_todo.pop(0))
            while cast_todo:
                self._cast(*cast_todo.pop(0))
            S.barrier()
            with nc.named_scope(f"ffn{l}"):
                S.emit()

    def _gates(self, lg, blg, gates, bgates, gw, gs):
        S = self.S
        m1, bm1 = gs["m1"]; m2, bm2 = gs["m2"]; s, bs = gs["s"]; s2, bs2 = gs["s2"]
        k1, bk1 = gw["m1"]; l2, bl2 = gw["l2"]; k2, bk2 = gw["m2"]
        bc8 = lambda ap: ap.unsqueeze(2).to_broadcast([128, 4, 8])
        S.op("dve", lambda e: e.tensor_reduce(out=m1[:], in_=lg[:], axis=AX.X, op=ALU.max), reads=[blg], writes=[bm1])
        S.op("dve", lambda e: e.tensor_tensor(out=k1[:], in0=lg[:], in1=bc8(m1[:]), op=ALU.is_equal), reads=[blg, bm1], writes=[bk1])
        S.op("dve", lambda e: e.scalar_tensor_tensor(out=l2[:], in0=k1[:], scalar=-1e30, in1=lg[:], op0=ALU.mult, op1=ALU.add), reads=[bk1, blg], writes=[bl2])
        S.op("dve", lambda e: e.tensor_reduce(out=m2[:], in_=l2[:], axis=AX.X, op=ALU.max), reads=[bl2], writes=[bm2])
        S.op("dve", lambda e: e.tensor_tensor(out=k2[:], in0=l2[:], in1=bc8(m2[:]), op=ALU.is_equal), reads=[bl2, bm2], writes=[bk2])
        S.op("dve", lambda e: e.tensor_tensor(out=s[:], in0=m1[:], in1=m2[:], op=ALU.subtract), reads=[bm1, bm2], writes=[bs])
        S.op("act", lambda e: e.activation(out=s[:], in_=s[:], func=AF.Sigmoid), reads=[bs], writes=[bs])
        S.op("dve", lambda e: e.tensor_scalar(out=s2[:], in0=s[:], scalar1=-1.0, scalar2=1.0, op0=ALU.mult, op1=ALU.add), reads=[bs], writes=[bs2])
        S.op("dve", lambda e: e.tensor_tensor(out=k1[:], in0=k1[:], in1=bc8(s[:]), op=ALU.mult), reads=[bk1, bs], writes=[bk1])
        S.op("dve", lambda e: e.tensor_tensor(out=k2[:], in0=k2[:], in1=bc8(s2[:]), op=ALU.mult), reads=[bk2, bs2], writes=[bk2])
        S.op("dve", lambda e: e.tensor_tensor(out=gates[:], in0=k1[:], in1=k2[:], op=ALU.add), reads=[bk1, bk2], writes=[bgates])


_CACHE = {}


def _get_prog(debug=False, nlayers=2, stop_after=None, layers=None):
    key = (debug, nlayers, stop_after, layers)
    if key not in _CACHE:
        p = Prog(debug=debug, nlayers=nlayers, stop_after=stop_after, layers=layers)
        _CACHE[key] = p.build()
    return _CACHE[key]


def _run(inputs, debug=False, nlayers=2, stop_after=None, trace=False, layers=None):
    nc = _get_prog(debug, nlayers, stop_after, layers)
    lay = _layout_inputs(inputs)
    lay["cstf"], lay["cstb"] = _constants()
    x = np.ascontiguousarray(inputs["x"], dtype=np.float32)
    in_maps = []
    for b in range(8):
        m = dict(lay)
        m["x"] = x[b]
        in_maps.append(m)
    kw = {"trace": True} if trace else {}
    return run_bass_kernel_spmd(nc, in_maps, core_ids=list(range(8)), **kw)


def kernel(**inputs):
    res = _run(inputs)
    return np.stack([np.asarray(r["out"], dtype=np.float32) for r in res.results], axis=0)
```
